# Optimizing a Trainium2 kernel written in Bass

```python
import math
import jax, jax.numpy as jnp
from jax import lax
import numpy as np

D_MODEL = 1024
BATCH = 8
SEQ = 4096
DEPTH = 4

GRID_W = 64
F32 = jnp.float32
EPS = 1e-6
HY_WIDTH = D_MODEL // 4
NA_HEAD_DIM = 64
NA_WIDTH = D_MODEL // 2
NA_HEADS = NA_WIDTH // NA_HEAD_DIM
POOL_WIDTH = D_MODEL // 4
POOL_WINDOWS = (2, 4, 8, 16)
POOL_GROUPS = len(POOL_WINDOWS)
POOL_GROUP_DIM = POOL_WIDTH // POOL_GROUPS
IN_WIDTH = 3 * HY_WIDTH + 3 * NA_WIDTH + POOL_WIDTH
SHORT_CONV = 3
FILTER_EMB = 33
FILTER_BANDS = (FILTER_EMB - 1) // 2
FILTER_HIDDEN = 64
DECAY_FAST = 0.3
DECAY_SLOW = 1.5
DECAY_TARGET = 1e-2
NA_KH_MAX = 8
NA_KW = 16
N_EXPERTS = 16
EC_CAPACITY = 2
EXPERT_FF = 2 * D_MODEL

kernel_name = 'hybrid_hyena_natten_pool_ecmoe_encoder'


def rms_norm(x, g):
    xf = x.astype(F32)
    y = xf * lax.rsqrt(jnp.mean(xf * xf, axis=-1, keepdims=True) + EPS)
    return (y * g.astype(F32)).astype(x.dtype)


def short_conv(u, w, b):
    up = jnp.pad(u, ((0, 0), (1, 1), (0, 0)))
    return up[:, :-2] * w[0] + up[:, 1:-1] * w[1] + up[:, 2:] * w[2] + b


def hyena_filter(L, w1, b1, w2, b2, w_out, freq):
    t = jnp.linspace(0.0, 1.0, L, dtype=F32)[:, None]
    ang = 2.0 * math.pi * jnp.arange(L, dtype=F32)[:, None] / L
    f = jnp.linspace(1e-4, FILTER_BANDS - 1, FILTER_BANDS, dtype=F32)[None, :]
    z = jnp.concatenate([t, jnp.cos(f * ang), -jnp.sin(f * ang)], axis=-1)
    fr = freq.astype(F32)
    h = jnp.sin(fr * (z @ w1.astype(F32) + b1.astype(F32)))
    h = jnp.sin(fr * (h @ w2.astype(F32) + b2.astype(F32)))
    h = h @ w_out.astype(F32)
    deltas = jnp.abs(jnp.linspace(math.log(DECAY_TARGET) / DECAY_FAST,
                                  math.log(DECAY_TARGET) / DECAY_SLOW, HY_WIDTH, dtype=F32))
    decay = jnp.exp(-t * deltas)
    h = h * jnp.concatenate([decay, decay], axis=-1)
    h_fwd, h_bwd = h[:, :HY_WIDTH], h[:, HY_WIDTH:]
    return jnp.concatenate([h_fwd, jnp.zeros((1, HY_WIDTH), F32), h_bwd[1:][::-1]], axis=0)


def hyena_mixer(u, sw, sb, filt, skip):
    u = short_conv(u, sw, sb)
    x0, x1, v = jnp.split(u, 3, axis=-1)
    z = (v * x1).astype(F32)
    L = z.shape[1]
    Z = jnp.fft.rfft(z, n=2 * L, axis=1)
    Hf = jnp.fft.rfft(filt, n=2 * L, axis=0)
    y = jnp.fft.irfft(Z * Hf[None], n=2 * L, axis=1)[:, :L]
    y = y + z * skip.astype(F32)
    return y.astype(u.dtype) * x0


def neighbourhood_attention(q, k, v, rpb):
    B, L, _ = q.shape
    rows = L // GRID_W
    kh = min(NA_KH_MAX, rows)
    shp = (B, rows, GRID_W, NA_HEADS, NA_HEAD_DIM)
    qg = (q * (NA_HEAD_DIM ** -0.5)).reshape(shp).transpose(1, 0, 2, 3, 4)
    kg = k.reshape(shp)
    vg = v.reshape(shp)
    cols = jnp.arange(GRID_W)
    col_start = jnp.clip(cols - NA_KW // 2, 0, GRID_W - NA_KW)
    col_idx = col_start[:, None] + jnp.arange(NA_KW)[None, :]
    dc_idx = col_idx - cols[:, None] + NA_KW - 1

    def one_row(args):
        q_row, r = args
        rs = jnp.clip(r - kh // 2, 0, rows - kh)
        k_nb = lax.dynamic_slice_in_dim(kg, rs, kh, axis=1)[:, :, col_idx]
        v_nb = lax.dynamic_slice_in_dim(vg, rs, kh, axis=1)[:, :, col_idx]
        s = jnp.einsum('bchd,bicjhd->bhcij', q_row, k_nb).astype(F32)
        dr_idx = rs + jnp.arange(kh) - r + NA_KH_MAX - 1
        bias = rpb[:, dr_idx][:, :, dc_idx].transpose(0, 2, 1, 3)
        s = s + bias.astype(F32)[None]
        p = jax.nn.softmax(s.reshape(B, NA_HEADS, GRID_W, kh * NA_KW), axis=-1)
        p = p.reshape(s.shape).astype(v_nb.dtype)
        return jnp.einsum('bhcij,bicjhd->bchd', p, v_nb)

    out = lax.map(one_row, (qg, jnp.arange(rows)))
    return out.transpose(1, 0, 2, 3, 4).reshape(B, L, NA_WIDTH)


def pool_mixer(u, pool_w, pool_scale):
    B, L, _ = u.shape
    ug = u.reshape(B, L, POOL_GROUPS, POOL_GROUP_DIM).astype(F32)
    S = jnp.concatenate([jnp.zeros((B, 1, POOL_GROUPS, POOL_GROUP_DIM), F32),
                         jnp.cumsum(ug, axis=1)], axis=1)
    t = jnp.arange(L)
    means = []
    for g, w in enumerate(POOL_WINDOWS):
        lo = jnp.clip(t - w // 2, 0, L)
        hi = jnp.clip(t + w // 2, 0, L)
        cnt = (hi - lo).astype(F32)[None, :, None]
        means.append((S[:, hi, g] - S[:, lo, g]) / cnt)
    pooled = jnp.stack(means, axis=2) - ug
    y = jnp.einsum('blgc,gcd->blgd', pooled, pool_w.astype(F32)).reshape(B, L, POOL_WIDTH)
    return (y * pool_scale.astype(F32)).astype(u.dtype)


def expert_choice_moe(h, w_router, w_gate, w_up, w_down):
    B, L, D = h.shape
    cap = EC_CAPACITY * L // N_EXPERTS
    aff = jax.nn.softmax(h.astype(F32) @ w_router.astype(F32), axis=-1)
    gate, idx = lax.top_k(aff.transpose(0, 2, 1), cap)
    xs = jax.vmap(lambda hb, ib: hb[ib])(h, idx)
    a = jnp.einsum('becd,edf->becf', xs, w_gate)
    b = jnp.einsum('becd,edf->becf', xs, w_up)
    y = jnp.einsum('becf,efd->becd', jax.nn.silu(a) * b, w_down)
    y = y * gate.astype(y.dtype)[..., None]
    return jax.vmap(lambda yb, ib: jnp.zeros((L, D), yb.dtype).at[ib.reshape(-1)].add(
        yb.reshape(-1, D)))(y, idx)


def setup_inputs(seed: int = 0) -> dict:
    key = jax.random.key(seed)
    ks = jax.random.split(key, 24)
    n = lambda k, s, sc: jax.random.normal(k, s, F32) * sc
    gain = lambda k, s: 1.0 + 0.02 * jax.random.normal(k, s, F32)
    L_ = DEPTH
    return {
        'x': jax.random.normal(ks[0], (BATCH, SEQ, D_MODEL), F32),
        'norm1_g': gain(ks[1], (L_, D_MODEL)),
        'w_in': n(ks[2], (L_, D_MODEL, IN_WIDTH), D_MODEL ** -0.5),
        'hy_short_w': n(ks[3], (L_, SHORT_CONV, 3 * HY_WIDTH), SHORT_CONV ** -0.5),
        'hy_short_b': n(ks[4], (L_, 3 * HY_WIDTH), 0.02),
        'hy_f_w1': n(ks[5], (L_, FILTER_EMB, FILTER_HIDDEN), FILTER_EMB ** -0.5),
        'hy_f_b1': n(ks[6], (L_, FILTER_HIDDEN), 0.02),
        'hy_f_w2': n(ks[7], (L_, FILTER_HIDDEN, FILTER_HIDDEN), FILTER_HIDDEN ** -0.5),
        'hy_f_b2': n(ks[8], (L_, FILTER_HIDDEN), 0.02),
        'hy_f_wout': n(ks[9], (L_, FILTER_HIDDEN, 2 * HY_WIDTH), FILTER_HIDDEN ** -0.5),
        'hy_f_freq': gain(ks[10], (L_, FILTER_HIDDEN)),
        'hy_skip': n(ks[11], (L_, HY_WIDTH), 1.0),
        'na_rpb': n(ks[12], (L_, NA_HEADS, 2 * NA_KH_MAX - 1, 2 * NA_KW - 1), 0.02),
        'pool_w': n(ks[13], (L_, POOL_GROUPS, POOL_GROUP_DIM, POOL_GROUP_DIM), POOL_GROUP_DIM ** -0.5),
        'pool_scale': 1.0 + 0.1 * jax.random.normal(ks[14], (L_, POOL_WIDTH), F32),
        'mix_norm_g': gain(ks[15], (L_, D_MODEL)),
        'w_out': n(ks[16], (L_, D_MODEL, D_MODEL), D_MODEL ** -0.5),
        'norm2_g': gain(ks[17], (L_, D_MODEL)),
        'w_router': n(ks[18], (L_, D_MODEL, N_EXPERTS), D_MODEL ** -0.5),
        'w_gate': n(ks[19], (L_, N_EXPERTS, D_MODEL, EXPERT_FF), D_MODEL ** -0.5),
        'w_up': n(ks[20], (L_, N_EXPERTS, D_MODEL, EXPERT_FF), D_MODEL ** -0.5),
        'w_down': n(ks[21], (L_, N_EXPERTS, EXPERT_FF, D_MODEL), EXPERT_FF ** -0.5),
        'final_g': gain(ks[22], (D_MODEL,)),
    }


def reference(x, norm1_g, w_in, hy_short_w, hy_short_b, hy_f_w1, hy_f_b1, hy_f_w2, hy_f_b2,
              hy_f_wout, hy_f_freq, hy_skip, na_rpb, pool_w, pool_scale, mix_norm_g, w_out,
              norm2_g, w_router, w_gate, w_up, w_down, final_g):
    L = x.shape[1]
    cuts = [3 * HY_WIDTH, 3 * HY_WIDTH + NA_WIDTH, 3 * HY_WIDTH + 2 * NA_WIDTH,
            3 * HY_WIDTH + 3 * NA_WIDTH]
    for i in range(DEPTH):
        h = rms_norm(x, norm1_g[i])
        proj = h @ w_in[i]
        hy_in, q, k, v, pool_in = jnp.split(proj, cuts, axis=-1)
        filt = hyena_filter(L, hy_f_w1[i], hy_f_b1[i], hy_f_w2[i], hy_f_b2[i],
                            hy_f_wout[i], hy_f_freq[i])
        y_hy = hyena_mixer(hy_in, hy_short_w[i], hy_short_b[i], filt, hy_skip[i])
        y_na = neighbourhood_attention(q, k, v, na_rpb[i])
        y_pool = pool_mixer(pool_in, pool_w[i], pool_scale[i])
        g = mix_norm_g[i]
        mixed = jnp.concatenate([
            rms_norm(y_hy, g[:HY_WIDTH]),
            rms_norm(y_na, g[HY_WIDTH:HY_WIDTH + NA_WIDTH]),
            rms_norm(y_pool, g[HY_WIDTH + NA_WIDTH:])], axis=-1)
        x = x + mixed @ w_out[i]
        x = x + expert_choice_moe(rms_norm(x, norm2_g[i]), w_router[i], w_gate[i],
                                  w_up[i], w_down[i])
    return rms_norm(x, final_g)
```

```python
import math
import numpy as np
import ml_dtypes
from contextlib import ExitStack
import concourse.bass as bass
import concourse.mybir as mybir
from concourse.bass_utils import run_bass_kernel_spmd

F32 = mybir.dt.float32
BF16 = mybir.dt.bfloat16
I32 = mybir.dt.int32
AF = mybir.ActivationFunctionType
ALU = mybir.AluOpType
NPBF = ml_dtypes.bfloat16

L = 4096
D = 1024
NT = 32
DEPTH = 4
EPS = 1e-6
MAGIC = 12582912.0
NEG = -30000.0


class T:
    def __init__(self, t, key):
        self.t = t
        self.key = key

    def __getitem__(self, k):
        return self.t[k]


class Prog:
    ENG = ('sp', 'act', 'dve', 'pool', 'pe')

    def __init__(self, nc):
        self.nc = nc
        self.ops = []
        self.n_dma_sems = {'sp': 28, 'act': 4, 'pool': 28}

    def add(self, eng, fn, r=(), w=(), dma=False):
        self.ops.append((eng, fn, tuple(r), tuple(w), dma, ()))

    def barrier(self):
        self.ops.append(('BAR',))

    def _expand(self):
        new = []
        last_eng = {}
        dmas = []
        for op in self.ops:
            if op[0] == 'BAR':
                dep = set(last_eng.values()) | set(dmas)
                dmas = []
                for e in self.ENG:
                    new.append((e, (lambda en: en.nop()), (), (), False, tuple(dep)))
                    last_eng[e] = len(new) - 1
            else:
                new.append(op)
                last_eng[op[0]] = len(new) - 1
                if op[4]:
                    dmas.append(len(new) - 1)
        self.ops = new

    def emit(self):
        nc = self.nc
        self._expand()
        ops = self.ops
        n = len(ops)
        last_w = {}
        readers = {}
        deps = [None] * n
        for i, (eng, fn, r, w, dma, xd) in enumerate(ops):
            d = set(xd)
            for k in r:
                if k in last_w:
                    d.add(last_w[k])
            for k in w:
                if k in last_w:
                    d.add(last_w[k])
                for j in readers.get(k, ()):
                    d.add(j)
            d.discard(i)
            deps[i] = d
            for k in w:
                last_w[k] = i
                readers[k] = []
            for k in r:
                readers.setdefault(k, []).append(i)
        need_sig = [False] * n
        real_deps = [None] * n
        for i in range(n):
            eng_i, dma_i = ops[i][0], ops[i][4]
            rd = []
            for j in deps[i]:
                eng_j, dma_j = ops[j][0], ops[j][4]
                if dma_j or dma_i or eng_j != eng_i or eng_i != 'pe':
                    rd.append(j)
                    need_sig[j] = True
            real_deps[i] = rd
        with ExitStack() as st:
            csem = {e: st.enter_context(nc.semaphore('c_' + e)) for e in self.ENG}
            dsem = {e: [st.enter_context(nc.semaphore('d_%s%d' % (e, k))) for k in range(c)]
                    for e, c in self.n_dma_sems.items()}
            sig = [None] * n
            ccount = {e: 0 for e in csem}
            dcount = {e: [0] * len(v) for e, v in dsem.items()}
            drr = {e: 0 for e in dsem}
            prev_on_sem = [None] * n
            for i, (eng, fn, r, w, dma, xd) in enumerate(ops):
                if dma:
                    k = drr[eng]
                    drr[eng] = (k + 1) % len(dsem[eng])
                    if dcount[eng][k] > 0:
                        prev_on_sem[i] = (dsem[eng][k], dcount[eng][k])
                    dcount[eng][k] += 16
                    sig[i] = (dsem[eng][k], dcount[eng][k])
                elif need_sig[i]:
                    ccount[eng] += 1
                    sig[i] = (csem[eng], ccount[eng])
            streams = {e: [i for i in range(n) if ops[i][0] == e] for e in self.ENG}
            waited = {e: {} for e in self.ENG}

            def run(ename, e):
                for i in streams[ename]:
                    fn, dma = ops[i][1], ops[i][4]
                    ws = []
                    if prev_on_sem[i] is not None:
                        ws.append(prev_on_sem[i])
                    for j in real_deps[i]:
                        ws.append(sig[j])
                    best = {}
                    for s, v in ws:
                        if v > best.get(s.num, (None, 0))[1]:
                            best[s.num] = (s, v)
                    for s, v in best.values():
                        if waited[ename].get(s.num, 0) >= v:
                            continue
                        waited[ename][s.num] = v
                        e.wait_ge(s, v)
                    ins = fn(e)
                    if sig[i] is not None:
                        ins.then_inc(sig[i][0], 16 if dma else 1)

            with nc.Block() as block:
                @block.sync
                def _(e):
                    run('sp', e)

                @block.scalar
                def _(e):
                    run('act', e)

                @block.vector
                def _(e):
                    run('dve', e)

                @block.gpsimd
                def _(e):
                    run('pool', e)

                @block.tensor
                def _(e):
                    run('pe', e)
        return n


def _bf(a):
    return np.ascontiguousarray(np.asarray(a, np.float32).astype(NPBF))


def na_windows():
    rs = np.clip(np.arange(64) - 4, 0, 56)
    win = []
    for j in range(32):
        rows = [r for r in range(64) if rs[r] <= 2 * j + 1 and rs[r] + 7 >= 2 * j]
        rlo = min(rows) // 2 * 2
        rhi = max(rows) // 2 * 2 + 1
        win.append((rlo, rhi))
    return win


def na_type(j):
    return j if j < 4 else (4 if j <= 27 else 5 + (j - 28))


NA_TYPE_REP = [0, 1, 2, 3, 10, 28, 29, 30, 31]


def build_na_bias(rpb):
    win = na_windows()
    rs = np.clip(np.arange(64) - 4, 0, 56)
    cs = np.clip(np.arange(64) - 8, 0, 48)
    out = np.full((DEPTH, 8, 128, 9, 768), NEG, np.float32)
    kc = np.arange(64)
    c = np.arange(64)
    inwin_c = (kc[:, None] >= cs[None, :]) & (kc[:, None] < cs[None, :] + 16)
    dc = np.clip(kc[:, None] - c[None, :] + 15, 0, 30)
    for ti, j in enumerate(NA_TYPE_REP):
        rlo, rhi = win[j]
        r = np.arange(rlo, rhi + 1)
        kr = 2 * j + np.arange(2)
        inwin_r = (kr[:, None] >= rs[r][None, :]) & (kr[:, None] < rs[r][None, :] + 8)
        dr = np.clip(kr[:, None] - r[None, :] + 7, 0, 14)
        m = inwin_r[:, None, :, None] & inwin_c[None, :, None, :]
        tab = rpb[:, :, dr[:, None, :, None], dc[None, :, None, :]]
        tab = np.where(m[None, None], tab, NEG).reshape(DEPTH, 8, 128, len(r) * 64)
        out[:, :, :, ti, :len(r) * 64] = tab
    return _bf(out)


def build_consts():
    c = {}
    c['ident_bf'] = _bf(np.eye(128))
    c['ident_f'] = np.eye(128, dtype=np.float32)
    p = np.concatenate([np.arange(L), np.concatenate([[0], L - np.arange(1, L)])]).astype(np.float64)
    t = p / (L - 1)
    ang = 2.0 * math.pi * p / L
    f = np.linspace(1e-4, 15.0, 16)
    zf = np.concatenate([t[None, :], np.cos(f[:, None] * ang[None, :]), -np.sin(f[:, None] * ang[None, :])], axis=0)
    c['zfT'] = np.ascontiguousarray(zf.astype(np.float32))
    deltas = np.abs(np.linspace(math.log(1e-2) / 0.3, math.log(1e-2) / 1.5, 256))
    dec = np.exp(-t[:, None] * deltas[None, :])
    dec[L, :] = 0.0
    c['decayF'] = np.ascontiguousarray(dec.astype(np.float32))
    n1 = np.arange(128)[:, None]
    k1 = np.arange(128)[None, :]
    a1 = 2 * math.pi * n1 * k1 / 128.0
    c['c1ns1'] = _bf(np.stack([np.cos(a1), -np.sin(a1)], axis=1))
    a2 = 2 * math.pi * np.arange(128)[:, None] * np.arange(64)[None, :] / 128.0
    c['cbsb'] = _bf(np.stack([np.cos(a2), -np.sin(a2)], axis=1) / 8192.0)
    n2 = np.arange(64)
    k2 = np.arange(64)
    gall = np.zeros((128, 128, 3, 128), np.float64)
    for kk in range(128):
        th = 2 * math.pi * (n2[:, None] * k2[None, :] / 64.0 + n2[:, None] * kk / 8192.0)
        gr, gi = np.cos(th), -np.sin(th)
        gf = np.zeros((128, 128))
        gf[0:64, 0:64] = gr
        gf[64:128, 0:64] = -gi
        gf[0:64, 64:128] = gi
        gf[64:128, 64:128] = gr
        gall[:, kk, 0, :] = gf
        gall[:, kk, 1, :] = np.concatenate([gf[:, 64:128], gf[:, 0:64]], axis=1)
        ph = th.T
        ir, ii = np.cos(ph), np.sin(ph)
        gfi = np.zeros((128, 128))
        gfi[0:64, 0:64] = ir
        gfi[64:128, 0:64] = -ii
        gfi[0:64, 64:128] = ii
        gfi[64:128, 64:128] = ir
        gall[:, kk, 2, :] = gfi
    c['gall'] = _bf(gall)
    pa = np.zeros((128, 4, 5, 128), np.float64)
    tt = np.arange(L)
    for g, w in enumerate((2, 4, 8, 16)):
        lo = np.clip(tt - w // 2, 0, L)
        hi = np.clip(tt + w // 2, 0, L)
        cnt = (hi - lo).astype(np.float64)

        def blk(tile_t, tile_s):
            tg = tile_t * 128 + np.arange(128)
            sg = tile_s * 128 + np.arange(128)
            m = ((sg[:, None] >= lo[tg][None, :]) & (sg[:, None] < hi[tg][None, :])) / cnt[tg][None, :]
            m = m - (sg[:, None] == tg[None, :])
            return m
        pa[:, g, 0, :] = blk(5, 5)
        pa[:, g, 1, :] = blk(0, 0)
        pa[:, g, 2, :] = blk(31, 31)
        pa[:, g, 3, :] = blk(5, 4)
        pa[:, g, 4, :] = blk(5, 6)
    c['poola'] = _bf(pa)
    ut = (np.arange(128)[:, None] < np.arange(128)[None, :]).astype(np.float32)
    c['ut_bf'] = _bf(ut)
    c['ones_bf'] = _bf(np.ones((128, 128)))
    tok = np.zeros((128, 32, 2), np.float32)
    tok[:, :, 0] = np.arange(32)[None, :]
    tok[:, :, 1] = np.arange(128)[:, None]
    c['tokc'] = _bf(tok)
    c['iota512'] = np.ascontiguousarray(np.broadcast_to(np.arange(512, dtype=np.float32)[None, :], (128, 512)))
    return c


CONST_SPECS = None


def build_program(n_layers=DEPTH, dbg=None):
    nc = bass.Bass("TRN2", target_bir_lowering=False)
    P = Prog(nc)
    dbg = dbg or {}

    def din(name, shape, dt=F32):
        return nc.dram_tensor(name, list(shape), dt, kind="ExternalInput").ap()

    def dscr(name, shape, dt=F32):
        kind = "ExternalOutput" if name in dbg else "Internal"
        return nc.dram_tensor(name, list(shape), dt, kind=kind).ap()

    x_in = din('x', [L, D])
    W = {}
    for name, shape in [('norm1_g', [DEPTH, D]), ('w_in', [DEPTH, D, 2560]), ('hy_short_w', [DEPTH, 3, 768]),
                        ('hy_short_b', [DEPTH, 768]), ('hy_f_w1', [DEPTH, 33, 64]), ('hy_f_b1', [DEPTH, 64]),
                        ('hy_f_w2', [DEPTH, 64, 64]), ('hy_f_b2', [DEPTH, 64]), ('hy_f_wout', [DEPTH, 64, 512]),
                        ('hy_f_freq', [DEPTH, 64]), ('hy_skip', [DEPTH, 256]), ('pool_w', [DEPTH, 4, 64, 64]),
                        ('pool_scale', [DEPTH, 256]), ('mix_norm_g', [DEPTH, D]), ('w_out', [DEPTH, D, D]),
                        ('norm2_g', [DEPTH, D]), ('w_router', [DEPTH, D, 16]), ('w_gate', [DEPTH, 16, D, 2048]),
                        ('w_up', [DEPTH, 16, D, 2048]), ('w_down', [DEPTH, 16, 2048, D]), ('final_g', [1, D])]:
        W[name] = din(name, shape)
    nab = din('nab', [DEPTH, 8, 128, 9, 768], BF16)
    C = {}
    for name, shape, dt in [('ident_bf', [128, 128], BF16), ('ident_f', [128, 128], F32), ('zfT', [33, 8192], F32),
                            ('decayF', [8192, 256], F32), ('c1ns1', [128, 2, 128], BF16), ('cbsb', [128, 2, 64], BF16),
                            ('gall', [128, 128, 3, 128], BF16), ('poola', [128, 4, 5, 128], BF16),
                            ('ut_bf', [128, 128], BF16), ('ones_bf', [128, 128], BF16), ('tokc', [128, 32, 2], BF16),
                            ('iota512', [128, 512], F32)]:
        C[name] = din(name, shape, dt)
    out = nc.dram_tensor('out', [L, D], F32, kind="ExternalOutput").ap()

    xr = dscr('xr', [L, D])
    hyT_d = dscr('hyT_d', [768, L], BF16)
    qT_d = dscr('qT_d', [512, L], BF16)
    kT_d = dscr('kT_d', [512, L], BF16)
    v_d = dscr('v_d', [L, 520], BF16)
    pool_d = dscr('pool_d', [L, 256], BF16)
    z_d = dscr('z_d', [L, 256], BF16)
    x0_d = dscr('x0_d', [L, 256], BF16)
    filt_d = dscr('filt_d', [8192, 256], BF16)
    Yz_d = dscr('Yz_d', [2, 128, 64, 256], BF16)
    Yf_d = dscr('Yf_d', [2, 128, 64, 256], BF16)
    U_d = dscr('U_d', [2, 64, 128, 256], BF16)
    ymix_d = dscr('ymix_d', [L, D])
    h2_d = dscr('h2_d', [L, D], BF16)
    dbg_aff = dscr('dbg_aff', [128, 512]) if 'dbg_aff' in dbg else None
    dbg_idx = dscr('dbg_idx', [128, 64]) if 'dbg_idx' in dbg else None
    dbg_affT = dscr('dbg_affT', [16, L]) if 'dbg_affT' in dbg else None
    dbg_lo = dscr('dbg_lo', [16, 4]) if 'dbg_lo' in dbg else None
    dbg_mask = dscr('dbg_mask', [128, 512]) if 'dbg_mask' in dbg else None
    dbg_rh = dscr('dbg_rh', [128, 2048], BF16) if 'dbg_rh' in dbg else None
    dbg_sel = dscr('dbg_sel', [128, 512], BF16) if 'dbg_sel' in dbg else None
    dbg_idxg = dscr('dbg_idxg', [128, 16]) if 'dbg_idxg' in dbg else None
    dbg_maskT = dscr('dbg_maskT', [16, L]) if 'dbg_maskT' in dbg else None
    dbg_afftok = dscr('dbg_afftok', [128, 512]) if 'dbg_afftok' in dbg else None

    SB_BASE = 16640
    SB_END = 212000
    sb_off = [SB_BASE]
    uid = [0]

    def sb(shape, dt, name=None):
        uid[0] += 1
        nm = '%s_%d' % (name or 't', uid[0])
        esz = 4 if dt in (F32, I32) else 2
        sz = int(np.prod(shape[1:])) * esz
        sz = (sz + 63) // 64 * 64
        o = sb_off[0]
        assert o + sz <= SB_END, ('SBUF overflow', nm, o, sz)
        sb_off[0] = o + sz
        return T(nc.alloc_sbuf_tensor_at(nm, list(shape), dt, offset=o), nm)

    def mark():
        return sb_off[0]

    def release(m):
        sb_off[0] = m
        P.barrier()

    PS = []
    for i in range(8):
        PS.append(T(nc.alloc_psum_tensor('ps%d' % i, [128, 512], F32), 'ps%d' % i))
    ps_rr = [0]

    def ps_next():
        t = PS[ps_rr[0] % 8]
        ps_rr[0] += 1
        return t

    def dma(q, out_ap, in_ap, r, w, **kw):
        P.add(q, lambda e: e.dma_start(out=out_ap, in_=in_ap, **kw), r=r, w=w, dma=True)

    ident_bf = sb([128, 128], BF16, 'identbf')
    ident_f = sb([128, 128], F32, 'identf')
    dma('sp', ident_bf[:], C['ident_bf'], [], [ident_bf.key])
    dma('sp', ident_f[:], C['ident_f'], [], [ident_f.key])
    eps_t = sb([128, 1], F32, 'eps')
    P.add('dve', lambda e: e.memset(eps_t[:], EPS), w=[eps_t.key])
    persist_mark = mark()

    def bcast_rows(src_row_ap, n_part, width):
        return src_row_ap.broadcast_to([n_part, width])

    def rstd_from_ss(ss, n):
        P.add('act', lambda e: e.activation(ss[:], ss[:], AF.Sqrt, bias=eps_t[:], scale=1.0 / n), r=[ss.key, eps_t.key], w=[ss.key])
        P.add('dve', lambda e: e.reciprocal(ss[:], ss[:]), r=[ss.key], w=[ss.key])

    evac_rr = [0]

    def evac(out_ap, in_ap, r, w, scale=None):
        evac_rr[0] += 1
        if scale is not None:
            P.add('act', lambda e: e.mul(out_ap, in_ap, scale), r=r, w=w)
        elif evac_rr[0] % 2 == 0:
            P.add('act', lambda e: e.copy(out_ap, in_ap), r=r, w=w)
        else:
            P.add('dve', lambda e: e.tensor_copy(out_ap, in_ap), r=r, w=w)

    def stage_A(l):
        m0 = mark()
        xsrc = x_in if l == 0 else xr
        win = sb([128, 8, 2560], BF16, 'win')
        wv = W['w_in'][l].rearrange("(k p) n -> p k n", p=128)
        for k in range(8):
            dma('pool', win[:, k, :], wv[:, k, :], [], [(win.key, k)])
        g1b = sb([128, D], F32, 'g1b')
        dma('sp', g1b[:], bcast_rows(W['norm1_g'][l:l + 1, :], 128, D), [], [g1b.key])
        xts = [sb([128, D], F32, 'xt') for _ in range(2)]
        junk = sb([128, D], F32, 'junk')
        sss = [sb([128, 1], F32, 'ss') for _ in range(2)]
        hbs = [sb([128, D], BF16, 'hb') for _ in range(2)]
        hTs = [sb([128, 8, 512], BF16, 'hT') for _ in range(2)]
        fms = [sb([128, 14, 512], BF16, 'fm') for _ in range(2)]
        vts = [sb([128, 8, 65], BF16, 'vt') for _ in range(2)]
        pts = [sb([128, 256], BF16, 'pt') for _ in range(2)]
        for vt in vts:
            P.add('pool', lambda e, vt=vt: e.memset(vt[:], 1.0), w=[vt.key])
        tcount = 0
        for st in range(8):
            hT = hTs[st % 2]
            fm = fms[st % 2]
            for a in range(4):
                t = 4 * st + a
                xt = xts[tcount % 2]
                ss = sss[tcount % 2]
                hb = hbs[tcount % 2]
                tcount += 1
                dma('sp', xt[:], xsrc[128 * t:128 * t + 128, :], [('xr', t), 'xr_scatter'], [xt.key])
                P.add('act', lambda e, xt=xt, ss=ss: e.activation(junk[:], xt[:], AF.Square, accum_out=ss[:]),
                      r=[xt.key], w=[junk.key, ss.key])
                rstd_from_ss(ss, D)
                P.add('dve', lambda e, xt=xt, ss=ss, hb=hb: e.scalar_tensor_tensor(hb[:], xt[:], ss[:, 0:1], g1b[:], ALU.mult, ALU.mult),
                      r=[xt.key, ss.key, g1b.key], w=[hb.key])
                for k4 in range(2):
                    ps = ps_next()
                    psb = ps[:].bitcast(BF16)
                    for kk in range(4):
                        k = 4 * k4 + kk
                        P.add('pe', lambda e, psb=psb, hb=hb, k=k, kk=kk: e.transpose(psb[:, 128 * kk:128 * kk + 128], hb[:, 128 * k:128 * k + 128], ident_bf[:]),
                              r=[hb.key, ident_bf.key], w=[ps.key])
                    evac(hT[:, 4 * k4:4 * k4 + 4, 128 * a:128 * a + 128], psb[:, 0:512].rearrange("p (k t) -> p k t", k=4),
                         [ps.key], [(hT.key, a)])
            hkeys = [(hT.key, a) for a in range(4)]
            for cc in range(14):
                ps = ps_next()
                col = 128 * cc
                for k in range(8):
                    P.add('pe', lambda e, ps=ps, k=k, col=col, hT=hT: e.matmul(ps[:], win[:, k, col:col + 128], hT[:, k, :], start=(k == 0), stop=(k == 7)),
                          r=hkeys + [(win.key, k)], w=[ps.key])
                evac(fm[:, cc, :], ps[:], [ps.key], [(fm.key, cc)], scale=(0.125 if 6 <= cc < 10 else None))
            ts = slice(512 * st, 512 * st + 512)
            dma('sp', hyT_d.rearrange("(c p) t -> p c t", p=128)[:, :, ts], fm[:, 0:6, :], [(fm.key, c) for c in range(6)], [('hyT', st)])
            dma('sp', qT_d.rearrange("(c p) t -> p c t", p=128)[:, :, ts], fm[:, 6:10, :], [(fm.key, c) for c in range(6, 10)], [('qT', st)])
            dma('sp', kT_d.rearrange("(c p) t -> p c t", p=128)[:, :, ts], fm[:, 10:14, :], [(fm.key, c) for c in range(10, 14)], [('kT', st)])
            for a in range(4):
                t = 4 * st + a
                vt = vts[t % 2]
                pt = pts[t % 2]
                ps = ps_next()
                for k in range(8):
                    P.add('pe', lambda e, ps=ps, k=k, a=a, hT=hT: e.matmul(ps[:], hT[:, k, 128 * a:128 * a + 128], win[:, k, 1792:2304], start=(k == 0), stop=(k == 7)),
                          r=hkeys + [(win.key, k)], w=[ps.key])
                P.add('act', lambda e, ps=ps, vt=vt: e.copy(vt[:, :, 0:64], ps[:].rearrange("p (h d) -> p h d", h=8)), r=[ps.key], w=[vt.key])
                dma('sp', v_d[128 * t:128 * t + 128, :], vt[:].rearrange("p h d -> p (h d)"), [vt.key], [('v', t)])
                ps2 = ps_next()
                for k in range(8):
                    P.add('pe', lambda e, ps2=ps2, k=k, a=a, hT=hT: e.matmul(ps2[:, 0:256], hT[:, k, 128 * a:128 * a + 128], win[:, k, 2304:2560], start=(k == 0), stop=(k == 7)),
                          r=hkeys + [(win.key, k)], w=[ps2.key])
                P.add('dve', lambda e, ps2=ps2, pt=pt: e.tensor_copy(pt[:], ps2[:, 0:256]), r=[ps2.key], w=[pt.key])
                dma('sp', pool_d[128 * t:128 * t + 128, :], pt[:], [pt.key], [('pool', t)])
        release(m0)

    def stage_H(l):
        m0 = mark()
        swT = sb([128, 3, 6], F32, 'swT')
        sbT = sb([128, 6], F32, 'sbT')
        for k3 in range(3):
            dma('sp', swT[:, k3, :], W['hy_short_w'][l, k3:k3 + 1, :].rearrange("o (c p) -> p (o c)", p=128), [], [(swT.key, k3)], allow_slow_non_contiguous=True)
        dma('sp', sbT[:], W['hy_short_b'][l:l + 1, :].rearrange("o (c p) -> p (o c)", p=128), [], [sbT.key], allow_slow_non_contiguous=True)
        ztok = sb([128, 32, 256], BF16, 'ztok')
        x0tok = sb([128, 32, 256], BF16, 'x0tok')
        m1 = mark()
        hyb = [sb([128, 4098], BF16, 'hyb') for _ in range(3)]
        t1 = sb([128, L], F32, 't1')
        ua = sb([128, L], F32, 'ua')
        ub = sb([128, L], F32, 'ub')
        zT = sb([128, L], BF16, 'zT')
        x0T = sb([128, L], BF16, 'x0T')
        for hb_ in hyb:
            P.add('pool', lambda e, hb_=hb_: e.memset(hb_[:], 0.0), w=[hb_.key])
        hyview = hyT_d.rearrange("(c p) t -> p c t", p=128)

        def conv(eng, dst, src, c):
            P.add(eng, lambda e: e.tensor_scalar(t1[:] if dst is not t1 else dst[:], src[:, 0:L], swT[:, 0, c:c + 1], sbT[:, c:c + 1], ALU.mult, ALU.add),
                  r=[src.key, (swT.key, 0), sbT.key], w=[t1.key])
            P.add(eng, lambda e: e.scalar_tensor_tensor(t1[:], src[:, 1:L + 1], swT[:, 1, c:c + 1], t1[:], ALU.mult, ALU.add),
                  r=[src.key, (swT.key, 1), t1.key], w=[t1.key])
            P.add(eng, lambda e: e.scalar_tensor_tensor(dst[:], src[:, 2:L + 2], swT[:, 2, c:c + 1], t1[:], ALU.mult, ALU.add),
                  r=[src.key, (swT.key, 2), t1.key], w=[dst.key])
        for pi in range(2):
            for which in range(3):
                dma('sp', hyb[which][:, 1:L + 1], hyview[:, 2 * which + pi, :], [('hyT', s) for s in range(8)], [hyb[which].key])
            conv('dve', x0T, hyb[0], pi)
            conv('dve', ua, hyb[1], 2 + pi)
            conv('dve', ub, hyb[2], 4 + pi)
            P.add('dve', lambda e: e.tensor_tensor(zT[:], ua[:], ub[:], ALU.mult), r=[ua.key, ub.key], w=[zT.key])
            for src, dst in ((zT, ztok), (x0T, x0tok)):
                for t4 in range(8):
                    ps = ps_next()
                    psb = ps[:].bitcast(BF16)
                    for q in range(4):
                        tt = 4 * t4 + q
                        P.add('pe', lambda e, psb=psb, src=src, tt=tt, q=q: e.transpose(psb[:, 128 * q:128 * q + 128], src[:, 128 * tt:128 * tt + 128], ident_bf[:]),
                              r=[src.key, ident_bf.key], w=[ps.key])
                    evac(dst[:, 4 * t4:4 * t4 + 4, 128 * pi:128 * pi + 128], psb[:, 0:512].rearrange("p (q c) -> p q c", q=4), [ps.key], [(dst.key, pi)])
        dma('sp', z_d.rearrange("(t p) c -> p t c", p=128), ztok[:], [(ztok.key, 0), (ztok.key, 1)], ['z_d'])
        dma('sp', x0_d.rearrange("(t p) c -> p t c", p=128), x0tok[:], [(x0tok.key, 0), (x0tok.key, 1)], ['x0_d'])
        release(m1)
        w1 = sb([33, 64], F32, 'w1')
        w2 = sb([64, 64], F32, 'w2')
        wo = sb([64, 512], F32, 'wo')
        fb = sb([64, 4], F32, 'fb')
        frb = sb([64, 2], F32, 'frb')
        dma('sp', w1[:], W['hy_f_w1'][l], [], [w1.key])
        dma('sp', w2[:], W['hy_f_w2'][l], [], [w2.key])
        dma('sp', wo[:], W['hy_f_wout'][l], [], [wo.key])
        dma('sp', fb[:, 0:1], W['hy_f_b1'][l:l + 1, :].rearrange("o p -> p o"), [], [(fb.key, 0)], allow_slow_non_contiguous=True)
        dma('sp', fb[:, 1:2], W['hy_f_b2'][l:l + 1, :].rearrange("o p -> p o"), [], [(fb.key, 1)], allow_slow_non_contiguous=True)
        dma('sp', fb[:, 2:3], W['hy_f_freq'][l:l + 1, :].rearrange("o p -> p o"), [], [(fb.key, 2)], allow_slow_non_contiguous=True)
        P.add('dve', lambda e: e.tensor_scalar(frb[:], fb[:, 0:2], fb[:, 2:3], None, ALU.mult), r=[(fb.key, 0), (fb.key, 1), (fb.key, 2)], w=[frb.key])
        zfs = [sb([33, 512], F32, 'zf') for _ in range(2)]
        arg = sb([64, 512], F32, 'arg')
        red = sb([64, 512], F32, 'red')
        h1 = sb([64, 512], F32, 'h1')
        h2 = sb([64, 512], F32, 'h2')
        decs = [sb([128, 4, 256], F32, 'dec') for _ in range(2)]
        fsts = [sb([128, 4, 256], BF16, 'fst') for _ in range(2)]

        def sin_layer(ps, dst, bcol):
            P.add('dve', lambda e: e.tensor_scalar(arg[:], ps[0:64, :], fb[:, 2:3], frb[:, bcol:bcol + 1], ALU.mult, ALU.add),
                  r=[ps.key, (fb.key, 2), frb.key], w=[arg.key])
            P.add('dve', lambda e: e.tensor_scalar(red[:], arg[:], 1.0 / (2 * math.pi), MAGIC, ALU.mult, ALU.add), r=[arg.key], w=[red.key])
            P.add('dve', lambda e: e.tensor_scalar(red[:], red[:], -MAGIC, -2 * math.pi, ALU.add, ALU.mult), r=[red.key], w=[red.key])
            P.add('dve', lambda e: e.tensor_tensor(red[:], red[:], arg[:], ALU.add), r=[red.key, arg.key], w=[red.key])
            P.add('act', lambda e: e.activation(dst[:], red[:], AF.Sin), r=[red.key], w=[dst.key])
        for pc in range(16):
            zf = zfs[pc % 2]
            dec = decs[pc % 2]
            fst = fsts[pc % 2]
            dma('sp', zf[:], C['zfT'][:, 512 * pc:512 * pc + 512], [], [zf.key])
            dma('sp', dec[:], C['decayF'][512 * pc:512 * pc + 512, :].rearrange("(q p) c -> p q c", p=128), [], [dec.key])
            ps = ps_next()
            P.add('pe', lambda e, ps=ps, zf=zf: e.matmul(ps[0:64, :], w1[:], zf[:], start=True, stop=True), r=[w1.key, zf.key], w=[ps.key])
            sin_layer(ps, h1, 0)
            ps = ps_next()
            P.add('pe', lambda e, ps=ps: e.matmul(ps[0:64, :], w2[:], h1[:], start=True, stop=True), r=[w2.key, h1.key], w=[ps.key])
            sin_layer(ps, h2, 1)
            half = 0 if pc < 8 else 1
            for qq in range(2):
                ps = ps_next()
                for q2 in range(2):
                    q = 2 * qq + q2
                    P.add('pe', lambda e, ps=ps, q=q, q2=q2, half=half: e.matmul(ps[:, 256 * q2:256 * q2 + 256], h2[:, 128 * q:128 * q + 128], wo[:, 256 * half:256 * half + 256], start=True, stop=True),
                          r=[h2.key, wo.key], w=[ps.key])
                P.add('dve', lambda e, ps=ps, qq=qq, dec=dec, fst=fst: e.tensor_tensor(fst[:, 2 * qq:2 * qq + 2, :], ps[:].rearrange("p (q c) -> p q c", q=2), dec[:, 2 * qq:2 * qq + 2, :], ALU.mult),
                      r=[ps.key, dec.key], w=[(fst.key, qq)])
            dma('sp', filt_d[512 * pc:512 * pc + 512, :].rearrange("(q p) c -> p q c", p=128), fst[:], [(fst.key, 0), (fst.key, 1)], [('filt', pc)])
        release(m1)
        c1 = sb([128, 2, 128], BF16, 'c1')
        dma('sp', c1[:], C['c1ns1'], [], [c1.key])
        z_sb = sb([64, 16384], BF16, 'z_sb')
        dma('sp', z_sb[:], z_d.rearrange("(n1 n2) c -> n1 (n2 c)", n2=64), ['z_d'], [z_sb.key])
        m2 = mark()
        f_sb = sb([128, 16384], BF16, 'f_sb')
        dma('sp', f_sb[:], filt_d.rearrange("(n1 n2) c -> n1 (n2 c)", n2=64), [('filt', pc) for pc in range(16)], [f_sb.key])
        ysts = [sb([128, 2048], BF16, 'yst') for _ in range(4)]
        yc = 0
        for src, K, dst_d, nm in ((z_sb, 64, Yz_d, 'Yz'), (f_sb, 128, Yf_d, 'Yf')):
            for grp in range(8):
                for ri in range(2):
                    yst = ysts[yc % 4]
                    yc += 1
                    for c4 in range(4):
                        ch = 4 * grp + c4
                        ps = ps_next()
                        P.add('pe', lambda e, ps=ps, src=src, K=K, ri=ri, ch=ch: e.matmul(ps[:], c1[0:K, ri, :], src[0:K, 512 * ch:512 * ch + 512], start=True, stop=True),
                              r=[c1.key, src.key], w=[ps.key])
                        evac(yst[:, 512 * c4:512 * c4 + 512], ps[:], [ps.key], [(yst.key, c4)])
                    dma('sp', dst_d[ri].rearrange("k n c -> k (n c)")[:, 2048 * grp:2048 * grp + 2048], yst[:], [(yst.key, c4) for c4 in range(4)], [(nm, ri, grp)])
        release(m2)
        galls = [sb([128, 16, 3, 128], BF16, 'gall') for _ in range(2)]
        yzs = [sb([128, 16, 256], BF16, 'yzg') for _ in range(2)]
        yfs = [sb([128, 16, 256], BF16, 'yfg') for _ in range(2)]
        usts = [sb([128, 16, 256], BF16, 'ust') for _ in range(2)]
        has = [sb([128, 512], BF16, 'ha') for _ in range(2)]
        hbs_ = [sb([128, 512], BF16, 'hb_') for _ in range(2)]
        t1s = [sb([128, 512], BF16, 'p1') for _ in range(2)]
        t2s = [sb([128, 512], BF16, 'p2') for _ in range(2)]
        allY = lambda nm: [(nm, ri, g) for ri in range(2) for g in range(8)]
        for g in range(8):
            ga = galls[g % 2]
            yz = yzs[g % 2]
            yf = yfs[g % 2]
            ust = usts[g % 2]
            dma('sp', ga[:], C['gall'][:, 16 * g:16 * g + 16, :, :], [], [ga.key])
            for ri in range(2):
                dma('sp', yz[64 * ri:64 * ri + 64, :, :], Yz_d[ri, 16 * g:16 * g + 16, :, :].rearrange("k n c -> n k c"), allY('Yz'), [(yz.key, ri)])
                dma('sp', yf[64 * ri:64 * ri + 64, :, :], Yf_d[ri, 16 * g:16 * g + 16, :, :].rearrange("k n c -> n k c"), allY('Yf'), [(yf.key, ri)])
            for kp in range(8):
                ha = has[kp % 2]
                hb_ = hbs_[kp % 2]
                p1 = t1s[kp % 2]
                p2 = t2s[kp % 2]
                psF, psFs, psX, psXs, psU = ps_next(), ps_next(), ps_next(), ps_next(), ps_next()
                for (pst, which, ysrc) in ((psF, 0, yf), (psFs, 1, yf), (psX, 0, yz), (psXs, 1, yz)):
                    for kq in range(2):
                        ki = 2 * kp + kq
                        P.add('pe', lambda e, pst=pst, which=which, ysrc=ysrc, ki=ki, kq=kq, ga=ga: e.matmul(pst[:, 256 * kq:256 * kq + 256], ga[:, ki, which, :], ysrc[:, ki, :], start=True, stop=True),
                              r=[ga.key, (ysrc.key, 0), (ysrc.key, 1)], w=[pst.key])
                P.add('act', lambda e, psF=psF, ha=ha: e.copy(ha[0:64, :], psF[0:64, :]), r=[psF.key], w=[(ha.key, 0)])
                P.add('act', lambda e, psFs=psFs, ha=ha: e.copy(ha[64:128, :], psFs[64:128, :]), r=[psFs.key], w=[(ha.key, 1)])
                P.add('act', lambda e, psFs=psFs, hb_=hb_: e.mul(hb_[0:64, :], psFs[0:64, :], -1.0), r=[psFs.key], w=[(hb_.key, 0)])
                P.add('act', lambda e, psF=psF, hb_=hb_: e.copy(hb_[64:128, :], psF[64:128, :]), r=[psF.key], w=[(hb_.key, 1)])
                P.add('dve', lambda e, psX=psX, ha=ha, p1=p1: e.tensor_tensor(p1[:], psX[:], ha[:], ALU.mult), r=[psX.key, (ha.key, 0), (ha.key, 1)], w=[p1.key])
                P.add('dve', lambda e, psXs=psXs, hb_=hb_, p2=p2: e.tensor_tensor(p2[:], psXs[:], hb_[:], ALU.mult), r=[psXs.key, (hb_.key, 0), (hb_.key, 1)], w=[p2.key])
                for kq in range(2):
                    ki = 2 * kp + kq
                    P.add('pe', lambda e, psU=psU, ki=ki, kq=kq, ga=ga, p1=p1: e.matmul(psU[:, 256 * kq:256 * kq + 256], ga[:, ki, 2, :], p1[:, 256 * kq:256 * kq + 256], start=True, stop=False),
                          r=[ga.key, p1.key], w=[psU.key])
                    P.add('pe', lambda e, psU=psU, ki=ki, kq=kq, ga=ga, p2=p2: e.matmul(psU[:, 256 * kq:256 * kq + 256], ga[:, ki, 2, :], p2[:, 256 * kq:256 * kq + 256], start=False, stop=True),
                          r=[ga.key, p2.key], w=[psU.key])
                P.add('act', lambda e, psU=psU, ust=ust, kp=kp: e.copy(ust[:, 2 * kp:2 * kp + 2, :], psU[:].rearrange("p (k c) -> p k c", k=2)), r=[psU.key], w=[(ust.key, kp)])
            for ri in range(2):
                dma('sp', U_d[ri, :, 16 * g:16 * g + 16, :], ust[64 * ri:64 * ri + 64, :, :], [(ust.key, kp) for kp in range(8)], [('U', ri, g)])
        release(m2)
        cb = sb([128, 2, 64], BF16, 'cb')
        dma('sp', cb[:], C['cbsb'], [], [cb.key])
        x0_sb = sb([64, 16384], BF16, 'x0_sb')
        dma('sp', x0_sb[:], x0_d.rearrange("(n1 n2) c -> n1 (n2 c)", n2=64), ['x0_d'], [x0_sb.key])
        skb = sb([64, 256], F32, 'skb')
        dma('sp', skb[:], bcast_rows(W['hy_skip'][l:l + 1, :], 64, 256), [], [skb.key])
        ubs = [sb([128, 2, 8, 256], BF16, 'ub') for _ in range(2)]
        youts = [sb([64, 8, 256], F32, 'yout') for _ in range(2)]
        zss = [sb([64, 512], F32, 'zs') for _ in range(2)]
        ys2 = [sb([64, 512], F32, 'ys2') for _ in range(2)]
        allU = [('U', ri, g) for ri in range(2) for g in range(8)]
        ymv = ymix_d.rearrange("(n1 n2) c -> n1 n2 c", n2=64)
        for ng in range(8):
            ub_ = ubs[ng % 2]
            yout = youts[ng % 2]
            for ri in range(2):
                dma('sp', ub_[:, ri, :, :], U_d[ri, 8 * ng:8 * ng + 8, :, :].rearrange("n k c -> k n c"), allU, [(ub_.key, ri)])
            for pr in range(4):
                ps = ps_next()
                zs = zss[pr % 2]
                y2 = ys2[pr % 2]
                n2a = 8 * ng + 2 * pr
                P.add('pe', lambda e, ps=ps, ub_=ub_, pr=pr: e.matmul(ps[0:64, :], cb[:, 0, :], ub_[:, 0, 2 * pr:2 * pr + 2, :].rearrange("p n c -> p (n c)"), start=True, stop=False),
                      r=[cb.key, (ub_.key, 0)], w=[ps.key])
                P.add('pe', lambda e, ps=ps, ub_=ub_, pr=pr: e.matmul(ps[0:64, :], cb[:, 1, :], ub_[:, 1, 2 * pr:2 * pr + 2, :].rearrange("p n c -> p (n c)"), start=False, stop=True),
                      r=[cb.key, (ub_.key, 1)], w=[ps.key])
                P.add('pool', lambda e, zs=zs, n2a=n2a: e.tensor_tensor(zs[:].rearrange("p (n c) -> p n c", n=2), z_sb[:, 256 * n2a:256 * n2a + 512].rearrange("p (n c) -> p n c", n=2),
                                                                          skb[:].unsqueeze(1).broadcast_to([64, 2, 256]), ALU.mult),
                      r=[z_sb.key, skb.key], w=[zs.key])
                P.add('dve', lambda e, ps=ps, zs=zs, y2=y2: e.tensor_tensor(y2[:], ps[0:64, :], zs[:], ALU.add), r=[ps.key, zs.key], w=[y2.key])
                P.add('dve', lambda e, y2=y2, yout=yout, pr=pr, n2a=n2a: e.tensor_tensor(yout[:, 2 * pr:2 * pr + 2, :].rearrange("p n c -> p (n c)"), y2[:], x0_sb[:, 256 * n2a:256 * n2a + 512], ALU.mult),
                      r=[y2.key, x0_sb.key], w=[(yout.key, pr)])
            dma('sp', ymv[:, 8 * ng:8 * ng + 8, 0:256], yout[:], [(yout.key, pr) for pr in range(4)], [('ymix_hy', ng)])
        release(m0)

    def stage_N(l):
        m0 = mark()
        win_ = na_windows()
        vaug = sb([128, 32, 520], BF16, 'vaug')
        dma('sp', vaug[:], v_d.rearrange("(t p) c -> p t c", p=128), [('v', t) for t in range(32)], [vaug.key])
        PT = sb([128, 32, 768], BF16, 'PT')
        qcs = [sb([128, L], BF16, 'qc') for _ in range(2)]
        kcs = [sb([128, L], BF16, 'kc') for _ in range(2)]
        nbs = [sb([128, 9, 768], BF16, 'nb') for _ in range(2)]
        yns = [sb([128, 32, 64], F32, 'yn') for _ in range(2)]
        rcs = [sb([128, 1], F32, 'rc') for _ in range(4)]
        rcc = 0
        qv = qT_d.rearrange("(c p) t -> p c t", p=128)
        kv = kT_d.rearrange("(c p) t -> p c t", p=128)
        for h in range(8):
            c, s = h // 2, h % 2
            qc, kc = qcs[c % 2], kcs[c % 2]
            if s == 0:
                dma('sp', qc[:], qv[:, c, :], [('qT', st) for st in range(8)], [qc.key])
                dma('sp', kc[:], kv[:, c, :], [('kT', st) for st in range(8)], [kc.key])
            nb = nbs[h % 2]
            dma('sp', nb[:], nab[l, h], [], [nb.key])
            yn = yns[h % 2]
            pb = 64 * s
            for j in range(32):
                rlo, rhi = win_[j]
                nq = (rhi - rlo + 1) * 64
                q0 = rlo * 64
                ty = na_type(j)
                off = 0
                while off < nq:
                    n = min(512, nq - off)
                    ps = ps_next()
                    P.add('pe', lambda e, ps=ps, kc=kc, qc=qc, pb=pb, j=j, q0=q0, off=off, n=n: e.matmul(ps[:, 0:n], kc[pb:pb + 64, 128 * j:128 * j + 128], qc[pb:pb + 64, q0 + off:q0 + off + n], start=True, stop=False),
                          r=[kc.key, qc.key], w=[ps.key])
                    P.add('pe', lambda e, ps=ps, nb=nb, ty=ty, off=off, n=n: e.matmul(ps[:, 0:n], ident_bf[:], nb[:, ty, off:off + n], start=False, stop=True),
                          r=[nb.key, ident_bf.key], w=[ps.key])
                    P.add('act', lambda e, ps=ps, j=j, off=off, n=n: e.activation(PT[:, j, off:off + n], ps[:, 0:n], AF.Exp), r=[ps.key], w=[(PT.key, j)])
                    off += n
            for i in range(32):
                js = [j for j in range(32) if win_[j][0] <= 2 * i and win_[j][1] >= 2 * i + 1]
                ps = ps_next()
                for n_, j in enumerate(js):
                    o = (2 * i - win_[j][0]) * 64
                    P.add('pe', lambda e, ps=ps, j=j, o=o, i=i, h=h, n_=n_, last=(n_ == len(js) - 1): e.matmul(ps[:, 0:65], PT[:, j, o:o + 128], vaug[:, j, 65 * h:65 * h + 65], start=(n_ == 0), stop=last),
                          r=[(PT.key, j), vaug.key], w=[ps.key])
                rc = rcs[rcc % 4]
                rcc += 1
                P.add('dve', lambda e, ps=ps, rc=rc: e.reciprocal(rc[:], ps[:, 64:65]), r=[ps.key], w=[rc.key])
                P.add('dve', lambda e, ps=ps, rc=rc, yn=yn, i=i: e.tensor_scalar(yn[:, i, :], ps[:, 0:64], rc[:, 0:1], None, ALU.mult), r=[ps.key, rc.key], w=[(yn.key, i)])
            dma('sp', ymix_d.rearrange("(t p) c -> p t c", p=128)[:, :, 256 + 64 * h:256 + 64 * h + 64], yn[:], [(yn.key, i) for i in range(32)], [('ymix_na', h)])
        release(m0)

    def stage_P(l):
        m0 = mark()
        u_sb = sb([128, 32, 256], BF16, 'u_sb')
        dma('sp', u_sb[:], pool_d.rearrange("(t p) c -> p t c", p=128), [('pool', t) for t in range(32)], [u_sb.key])
        pa = sb([128, 4, 5, 128], BF16, 'pa')
        dma('sp', pa[:], C['poola'], [], [pa.key])
        pw = sb([64, 4, 64], BF16, 'pw')
        dma('pool', pw[:], W['pool_w'][l].rearrange("g c d -> c g d"), [], [pw.key])
        psb_ = sb([128, 256], F32, 'pscale')
        dma('sp', psb_[:], bcast_rows(W['pool_scale'][l:l + 1, :], 128, 256), [], [psb_.key])
        pTs = [sb([64, 4, 128], BF16, 'pT') for _ in range(2)]
        yp = sb([128, 32, 256], F32, 'yp')
        for t in range(32):
            pT = pTs[t % 2]
            ps = ps_next()
            for g in range(4):
                srcs = []
                if t > 0:
                    srcs.append((t - 1, 3))
                srcs.append((t, 1 if t == 0 else (2 if t == 31 else 0)))
                if t < 31:
                    srcs.append((t + 1, 4))
                for n_, (tp, var) in enumerate(srcs):
                    P.add('pe', lambda e, ps=ps, g=g, tp=tp, var=var, n_=n_, last=(n_ == len(srcs) - 1): e.matmul(ps[0:64, 128 * g:128 * g + 128], u_sb[:, tp, 64 * g:64 * g + 64], pa[:, g, var, :], start=(n_ == 0), stop=last),
                          r=[u_sb.key, pa.key], w=[ps.key])
            P.add('act', lambda e, ps=ps, pT=pT: e.copy(pT[:].rearrange("p g t -> p (g t)"), ps[0:64, :]), r=[ps.key], w=[pT.key])
            ps2 = ps_next()
            for g in range(4):
                P.add('pe', lambda e, ps2=ps2, g=g, pT=pT: e.matmul(ps2[:, 64 * g:64 * g + 64], pT[:, g, :], pw[:, g, :], start=True, stop=True),
                      r=[pT.key, pw.key], w=[ps2.key])
            P.add('dve', lambda e, ps2=ps2, t=t: e.tensor_tensor(yp[:, t, :], ps2[:, 0:256], psb_[:], ALU.mult), r=[ps2.key, psb_.key], w=[(yp.key, t)])
        dma('sp', ymix_d.rearrange("(t p) c -> p t c", p=128)[:, :, 768:1024], yp[:], [(yp.key, t) for t in range(32)], ['ymix_pool'])
        release(m0)

    state = {}

    def stage_E(l):
        m0 = mark()
        aff_tok, affT = state['aff_tok'], state['affT']
        xsrc = x_in if l == 0 else xr
        wo_sb = sb([128, 8, D], BF16, 'wo_sb')
        wv = W['w_out'][l].rearrange("(k p) n -> p k n", p=128)
        for k in range(8):
            dma('pool', wo_sb[:, k, :], wv[:, k, :], [], [(wo_sb.key, k)])
        wr_sb = sb([128, 8, 16], F32, 'wr_sb')
        dma('sp', wr_sb[:], W['w_router'][l].rearrange("(k p) n -> p k n", p=128), [], [wr_sb.key])
        gmb = sb([128, D], F32, 'gmb')
        g2b = sb([128, D], F32, 'g2b')
        dma('sp', gmb[:], bcast_rows(W['mix_norm_g'][l:l + 1, :], 128, D), [], [gmb.key])
        dma('sp', g2b[:], bcast_rows(W['norm2_g'][l:l + 1, :], 128, D), [], [g2b.key])
        yms = [sb([128, D], F32, 'ym') for _ in range(2)]
        xts = [sb([128, D], F32, 'xt') for _ in range(2)]
        junk = sb([128, D], F32, 'junk')
        ss3 = [sb([128, 4], F32, 'ss3') for _ in range(2)]
        mxs = [sb([128, D], BF16, 'mx') for _ in range(2)]
        mTs = [sb([128, 8, 128], BF16, 'mT') for _ in range(2)]
        x1s = [sb([128, D], F32, 'x1') for _ in range(2)]
        h2fs = [sb([128, D], F32, 'h2f') for _ in range(2)]
        h2bs = [sb([128, D], BF16, 'h2b') for _ in range(2)]
        h2Ts = [sb([128, 8, 128], F32, 'h2T') for _ in range(2)]
        lgs = [sb([128, 16], F32, 'lg') for _ in range(2)]
        sm4 = [sb([128, 4], F32, 'sm4') for _ in range(2)]
        segs = [(0, 256), (256, 768), (768, 1024)]
        ymkeys = [('ymix_hy', ng) for ng in range(8)] + [('ymix_na', h) for h in range(8)] + ['ymix_pool']
        for t in range(32):
            b = t % 2
            ym, xt, s3, mx, mT, x1, h2f, h2b, h2T, lg, sm = yms[b], xts[b], ss3[b], mxs[b], mTs[b], x1s[b], h2fs[b], h2bs[b], h2Ts[b], lgs[b], sm4[b]
            rows = slice(128 * t, 128 * t + 128)
            dma('sp', ym[:], ymix_d[rows, :], ymkeys, [ym.key])
            dma('sp', xt[:], xsrc[rows, :], [('xr', t), 'xr_scatter'], [xt.key])
            for si, (a0, a1) in enumerate(segs):
                P.add('act', lambda e, ym=ym, s3=s3, si=si, a0=a0, a1=a1: e.activation(junk[:, a0:a1], ym[:, a0:a1], AF.Square, accum_out=s3[:, si:si + 1]),
                      r=[ym.key], w=[junk.key, (s3.key, si)])
                P.add('act', lambda e, s3=s3, si=si, n=a1 - a0: e.activation(s3[:, si:si + 1], s3[:, si:si + 1], AF.Sqrt, bias=eps_t[:], scale=1.0 / n), r=[(s3.key, si), eps_t.key], w=[(s3.key, si)])
                P.add('dve', lambda e, s3=s3, si=si: e.reciprocal(s3[:, si:si + 1], s3[:, si:si + 1]), r=[(s3.key, si)], w=[(s3.key, si)])
                P.add('dve', lambda e, ym=ym, s3=s3, mx=mx, si=si, a0=a0, a1=a1: e.scalar_tensor_tensor(mx[:, a0:a1], ym[:, a0:a1], s3[:, si:si + 1], gmb[:, a0:a1], ALU.mult, ALU.mult),
                      r=[ym.key, (s3.key, si), gmb.key], w=[(mx.key, si)])
            mxk = [(mx.key, si) for si in range(3)]
            for k4 in range(2):
                ps = ps_next()
                psb = ps[:].bitcast(BF16)
                for kk in range(4):
                    k = 4 * k4 + kk
                    P.add('pe', lambda e, psb=psb, mx=mx, k=k, kk=kk: e.transpose(psb[:, 128 * kk:128 * kk + 128], mx[:, 128 * k:128 * k + 128], ident_bf[:]),
                          r=mxk + [ident_bf.key], w=[ps.key])
                evac(mT[:, 4 * k4:4 * k4 + 4, :], psb[:, 0:512].rearrange("p (k t) -> p k t", k=4), [ps.key], [(mT.key, k4)])
            for nh in range(2):
                ps = ps_next()
                for k in range(8):
                    P.add('pe', lambda e, ps=ps, k=k, nh=nh, mT=mT: e.matmul(ps[:], mT[:, k, :], wo_sb[:, k, 512 * nh:512 * nh + 512], start=(k == 0), stop=(k == 7)),
                          r=[(mT.key, 0), (mT.key, 1), (wo_sb.key, k)], w=[ps.key])
                P.add('dve', lambda e, ps=ps, nh=nh, xt=xt, x1=x1: e.tensor_tensor(x1[:, 512 * nh:512 * nh + 512], ps[:], xt[:, 512 * nh:512 * nh + 512], ALU.add),
                      r=[ps.key, xt.key], w=[(x1.key, nh)])
            x1k = [(x1.key, 0), (x1.key, 1)]
            dma('sp', xr[rows, :], x1[:], x1k, [('xr', t)])
            P.add('act', lambda e, x1=x1, s3=s3: e.activation(junk[:], x1[:], AF.Square, accum_out=s3[:, 3:4]), r=x1k, w=[junk.key, (s3.key, 3)])
            P.add('act', lambda e, s3=s3: e.activation(s3[:, 3:4], s3[:, 3:4], AF.Sqrt, bias=eps_t[:], scale=1.0 / D), r=[(s3.key, 3), eps_t.key], w=[(s3.key, 3)])
            P.add('dve', lambda e, s3=s3: e.reciprocal(s3[:, 3:4], s3[:, 3:4]), r=[(s3.key, 3)], w=[(s3.key, 3)])
            P.add('dve', lambda e, x1=x1, s3=s3, h2f=h2f: e.scalar_tensor_tensor(h2f[:], x1[:], s3[:, 3:4], g2b[:], ALU.mult, ALU.mult), r=x1k + [(s3.key, 3), g2b.key], w=[h2f.key])
            P.add('pool', lambda e, h2f=h2f, h2b=h2b: e.tensor_copy(h2b[:], h2f[:]), r=[h2f.key], w=[h2b.key])
            dma('sp', h2_d[rows, :], h2b[:], [h2b.key], [('h2', t)])
            for k4 in range(2):
                ps = ps_next()
                for kk in range(4):
                    k = 4 * k4 + kk
                    P.add('pe', lambda e, ps=ps, h2f=h2f, k=k, kk=kk: e.transpose(ps[:, 128 * kk:128 * kk + 128], h2f[:, 128 * k:128 * k + 128], ident_f[:]),
                          r=[h2f.key, ident_f.key], w=[ps.key])
                evac(h2T[:, 4 * k4:4 * k4 + 4, :], ps[:].rearrange("p (k t) -> p k t", k=4), [ps.key], [(h2T.key, k4)])
            ps = ps_next()
            for k in range(8):
                P.add('pe', lambda e, ps=ps, k=k, h2T=h2T: e.matmul(ps[:, 0:16], h2T[:, k, :], wr_sb[:, k, :], start=(k == 0), stop=(k == 7)),
                      r=[(h2T.key, 0), (h2T.key, 1), wr_sb.key], w=[ps.key])
            P.add('dve', lambda e, ps=ps, lg=lg: e.tensor_copy(lg[:], ps[:, 0:16]), r=[ps.key], w=[lg.key])
            P.add('dve', lambda e, lg=lg, sm=sm: e.reduce_max(sm[:, 0:1], lg[:], mybir.AxisListType.X), r=[lg.key], w=[(sm.key, 0)])
            P.add('dve', lambda e, sm=sm: e.tensor_scalar(sm[:, 1:2], sm[:, 0:1], -1.0, None, ALU.mult), r=[(sm.key, 0)], w=[(sm.key, 1)])
            P.add('act', lambda e, lg=lg, sm=sm: e.activation(lg[:], lg[:], AF.Exp, bias=sm[:, 1:2], accum_out=sm[:, 2:3]), r=[lg.key, (sm.key, 1)], w=[lg.key, (sm.key, 2)])
            P.add('dve', lambda e, sm=sm: e.reciprocal(sm[:, 3:4], sm[:, 2:3]), r=[(sm.key, 2)], w=[(sm.key, 3)])
            P.add('dve', lambda e, lg=lg, sm=sm, t=t: e.tensor_scalar(aff_tok[:, t, :], lg[:], sm[:, 3:4], None, ALU.mult), r=[lg.key, (sm.key, 3)], w=[(aff_tok.key, t)])
            ps = ps_next()
            P.add('pe', lambda e, ps=ps, t=t: e.transpose(ps[0:16, 0:128], aff_tok[:, t, :], ident_f[:]), r=[(aff_tok.key, t), ident_f.key], w=[ps.key])
            P.add('act', lambda e, ps=ps, t=t: e.copy(affT[:, 128 * t:128 * t + 128], ps[0:16, 0:128]), r=[ps.key], w=[(affT.key, t)])
        release(m0)

    def stage_M(l):
        m0 = mark()
        aff_tok, affT = state['aff_tok'], state['affT']
        affk = [(affT.key, t) for t in range(32)]
        mask_f = sb([128, 512], F32, 'mask_f')
        mask_b = sb([128, 512], BF16, 'mask_b')
        mscope = mark()
        lo = sb([16, 1], F32, 'lo')
        hi = sb([16, 1], F32, 'hi')
        mid = sb([16, 1], F32, 'mid')
        cnt = sb([16, 1], F32, 'cnt')
        ge = sb([16, 1], F32, 'ge')
        dd = sb([16, 1], F32, 'dd')
        bjunk = sb([16, L], F32, 'bjunk')
        bk = 'bis'
        P.add('dve', lambda e: e.memset(lo[:], 0.0), w=[bk])
        P.add('dve', lambda e: e.memset(hi[:], 1.0), w=[bk])
        for it in range(32):
            P.add('dve', lambda e: e.tensor_tensor(mid[:], lo[:], hi[:], ALU.add), r=[bk], w=[bk])
            P.add('dve', lambda e: e.tensor_scalar(mid[:], mid[:], 0.5, None, ALU.mult), r=[bk], w=[bk])
            P.add('dve', lambda e: e.memset(cnt[:], 0.0), r=[bk], w=[bk])
            P.add('dve', lambda e: e.tensor_scalar(bjunk[:], affT[:], mid[:, 0:1], 0.0, ALU.is_ge, ALU.add, accum_out=cnt[:]), r=[bk] + (affk if it == 0 else []), w=[bk])
            P.add('dve', lambda e: e.tensor_single_scalar(ge[:], cnt[:], 512.0, ALU.is_ge), r=[bk], w=[bk])
            P.add('dve', lambda e: e.tensor_tensor(dd[:], mid[:], lo[:], ALU.subtract), r=[bk], w=[bk])
            P.add('dve', lambda e: e.scalar_tensor_tensor(lo[:], dd[:], ge[:, 0:1], lo[:], ALU.mult, ALU.add), r=[bk], w=[bk])
            P.add('dve', lambda e: e.tensor_tensor(dd[:], hi[:], mid[:], ALU.subtract), r=[bk], w=[bk])
            P.add('dve', lambda e: e.scalar_tensor_tensor(hi[:], dd[:], ge[:, 0:1], mid[:], ALU.mult, ALU.add), r=[bk], w=[bk])
        if dbg_affT is not None:
            dma('sp', dbg_affT, affT[:], [bk], ['dbg_affT'])
            dma('sp', dbg_lo[:, 0:1], lo[:], [bk], ['dbg_lo0'], allow_slow_non_contiguous=True)
            dma('sp', dbg_lo[:, 1:2], hi[:], [bk], ['dbg_lo1'], allow_slow_non_contiguous=True)
            dma('sp', dbg_lo[:, 2:3], cnt[:], [bk], ['dbg_lo2'], allow_slow_non_contiguous=True)
            dma('sp', dbg_lo[:, 3:4], ge[:], [bk], ['dbg_lo3'], allow_slow_non_contiguous=True)
            dma('sp', dbg_afftok, aff_tok[:].rearrange("p t e -> p (t e)"), [(aff_tok.key, t) for t in range(32)], ['dbg_afftok'])
        if dbg.get('mstop', 99) <= 1:
            release(m0)
            return
        maskT = sb([16, L], F32, 'maskT')
        P.add('dve', lambda e: e.tensor_scalar(maskT[:], affT[:], lo[:, 0:1], None, ALU.is_ge), r=[bk], w=[maskT.key])
        if dbg.get('mstop', 99) <= 1.25:
            release(m0)
            return
        ps = ps_next()
        for t in range(32):
            P.add('pe', lambda e, ps=ps, t=t: e.matmul(ps[:, 16 * t:16 * t + 16], maskT[:, 128 * t:128 * t + 128], ident_f[0:16, 0:16], start=True, stop=True), r=[maskT.key, ident_f.key], w=[ps.key])
        if dbg.get('mstop', 99) <= 1.5:
            P.add('dve', lambda e, ps=ps: e.tensor_copy(bjunk[:, 0:512], ps[0:16, :]), r=[ps.key], w=[bjunk.key])
            release(m0)
            return
        P.add('dve', lambda e, ps=ps: e.tensor_copy(mask_f[:], ps[:]), r=[ps.key], w=[mask_f.key])
        P.add('act', lambda e: e.copy(mask_b[:], mask_f[:]), r=[mask_f.key], w=[mask_b.key])
        if dbg_mask is not None:
            dma('sp', dbg_mask, mask_f[:], [mask_f.key], ['dbg_mask'])
            dma('sp', dbg_maskT, maskT[:], [maskT.key], ['dbg_maskT'])
        if dbg.get('mstop', 99) <= 2:
            release(m0)
            return
        release(mscope)
        ut = sb([128, 128], BF16, 'ut')
        ones = sb([128, 128], BF16, 'ones')
        dma('sp', ut[:], C['ut_bf'], [], [ut.key])
        dma('sp', ones[:], C['ones_bf'], [], [ones.key])
        ps_in = ps_next()
        ps_tot = ps_next()
        P.add('pe', lambda e: e.matmul(ps_in[:], ut[:], mask_b[:], start=True, stop=True), r=[ut.key, mask_b.key], w=[ps_in.key])
        P.add('pe', lambda e: e.matmul(ps_tot[:], ones[:], mask_b[:], start=True, stop=True), r=[ones.key, mask_b.key], w=[ps_tot.key])
        tot = sb([128, 32, 16], F32, 'tot')
        offs = sb([128, 32, 16], F32, 'offs')
        posm = sb([128, 32, 16], F32, 'posm')
        P.add('dve', lambda e: e.tensor_copy(tot[:].rearrange("p t e -> p (t e)"), ps_tot[:]), r=[ps_tot.key], w=[tot.key])
        P.add('dve', lambda e: e.memset(offs[:, 0, :], 0.0), w=[offs.key])
        for t in range(31):
            P.add('dve', lambda e, t=t: e.tensor_tensor(offs[:, t + 1, :], offs[:, t, :], tot[:, t, :], ALU.add), r=[offs.key, tot.key], w=[offs.key])
        pf = posm[:].rearrange("p t e -> p (t e)")
        P.add('dve', lambda e: e.tensor_tensor(pf, ps_in[:], offs[:].rearrange("p t e -> p (t e)"), ALU.add), r=[ps_in.key, offs.key], w=[posm.key])
        P.add('dve', lambda e: e.tensor_scalar(pf, pf, 1.0, None, ALU.add), r=[posm.key], w=[posm.key])
        P.add('dve', lambda e: e.tensor_tensor(pf, pf, mask_f[:], ALU.mult), r=[posm.key, mask_f.key], w=[posm.key])
        P.add('dve', lambda e: e.tensor_scalar(pf, pf, -1.0, None, ALU.add), r=[posm.key], w=[posm.key])
        if dbg.get('mstop', 99) <= 3:
            release(m0)
            return
        RH = sb([128, 32, 16, 4], BF16, 'RH')
        tokc = sb([128, 32, 2], BF16, 'tokc')
        dma('sp', tokc[:], C['tokc'], [], [tokc.key])
        ahi = sb([128, 512], BF16, 'ahi')
        ahf = sb([128, 512], F32, 'ahf')
        afk = [(aff_tok.key, t) for t in range(32)]
        aflat = aff_tok[:].rearrange("p t e -> p (t e)")
        P.add('dve', lambda e: e.tensor_copy(ahi[:], aflat), r=afk, w=[ahi.key])
        P.add('dve', lambda e: e.tensor_copy(ahf[:], ahi[:]), r=[ahi.key], w=[ahf.key])
        P.add('dve', lambda e: e.tensor_tensor(ahf[:], aflat, ahf[:], ALU.subtract), r=afk + [ahf.key], w=[ahf.key])
        for ee in range(16):
            P.add('pool', lambda e, ee=ee: e.tensor_copy(RH[:, :, ee, 0:2], tokc[:]), r=[tokc.key], w=[(RH.key, 'a', ee)])
        P.add('dve', lambda e: e.tensor_copy(RH[:, :, :, 2], ahi[:].rearrange("p (t e) -> p t e", t=32)), r=[ahi.key], w=[(RH.key, 'b')])
        P.add('dve', lambda e: e.tensor_copy(RH[:, :, :, 3], ahf[:].rearrange("p (t e) -> p t e", t=32)), r=[ahf.key], w=[(RH.key, 'c')])
        rhk = [(RH.key, 'a', ee) for ee in range(16)] + [(RH.key, 'b'), (RH.key, 'c')]
        if dbg.get('mstop', 99) <= 4:
            release(m0)
            return
        iota = sb([128, 512], F32, 'iota')
        dma('sp', iota[:], C['iota512'], [], [iota.key])
        if dbg_aff is not None:
            dma('sp', dbg_aff, posm[:].rearrange("p t e -> p (t e)"), [posm.key], ['dbg_aff'])
        sels = [sb([128, 512], BF16, 'sel') for _ in range(4)]
        idxg = sb([128, 16], F32, 'idxg')
        r4 = sb([4, 512], F32, 'r4')
        idxf = sb([128, 4], F32, 'idxf')
        idxis = [sb([128, 4], I32, 'idxi') for _ in range(2)]
        gates = [sb([128, 4], F32, 'gate') for _ in range(2)]
        xss = [sb([128, 4, D], BF16, 'xs') for _ in range(2)]
        xsT = sb([128, 8, 512], BF16, 'xsT')
        gT = sb([128, 16, 512], BF16, 'gT')
        sgs = [sb([128, 512], F32, 'sg') for _ in range(2)]
        yos = [sb([128, D], F32, 'yo') for _ in range(2)]
        NSLOT = 8
        wslots = [sb([128, 4096], BF16, 'wslot') for _ in range(NSLOT)]
        wrr = [0]

        def load_piece(src_ap, shape_str, **kw):
            slot = wslots[wrr[0] % NSLOT]
            wrr[0] += 1
            dst = slot[:].rearrange(shape_str, **kw)
            dma('pool', dst, src_ap, [], [slot.key])
            return slot, dst
        h2k = [('h2', t) for t in range(32)]
        selc = 0
        for ex in range(dbg.get('nexp', 16)):
            b = ex % 2
            idxi, gate, xs = idxis[b], gates[b], xss[b]
            ps_r = ps_next()
            for t in range(32):
                sel = sels[selc % 4]
                selc += 1
                P.add('dve' if t % 2 == 0 else 'pool', lambda e, sel=sel, t=t, ex=ex: e.tensor_scalar(sel[:], iota[:], posm[:, t, ex:ex + 1], None, ALU.is_equal),
                      r=[iota.key, posm.key], w=[sel.key])
                P.add('pe', lambda e, sel=sel, t=t, ex=ex, ps_r=ps_r: e.matmul(ps_r[0:4, :], RH[:, t, ex, :], sel[:], start=(t == 0), stop=(t == 31)),
                      r=[sel.key] + (rhk if t == 0 else []), w=[ps_r.key])
            P.add('dve', lambda e, ps_r=ps_r: e.tensor_copy(r4[:], ps_r[0:4, :]), r=[ps_r.key], w=[r4.key])
            ps_idx = ps_next()
            for j in range(4):
                P.add('pe', lambda e, j=j, ps_idx=ps_idx: e.matmul(ps_idx[:, 4 * j:4 * j + 4], r4[:, 128 * j:128 * j + 128], ident_f[0:4, 0:4], start=True, stop=True),
                      r=[r4.key, ident_f.key], w=[ps_idx.key])
            P.add('dve', lambda e, ps_idx=ps_idx: e.tensor_copy(idxg[:], ps_idx[:, 0:16]), r=[ps_idx.key], w=[idxg.key])
            if dbg_rh is not None and ex == 0:
                dma('sp', dbg_rh, RH[:].rearrange("p t e c -> p (t e c)"), rhk, ['dbg_rh'])
                dma('sp', dbg_idxg, idxg[:], [idxg.key], ['dbg_idxg'])
            ig = idxg[:].rearrange("p (j c) -> p j c", c=4)
            P.add('dve', lambda e, ig=ig: e.scalar_tensor_tensor(idxf[:], ig[:, :, 0], 128.0, ig[:, :, 1], ALU.mult, ALU.add), r=[idxg.key], w=[idxf.key])
            P.add('dve', lambda e: e.tensor_scalar(idxf[:], idxf[:], 0.0, float(L - 1), ALU.max, ALU.min), r=[idxf.key], w=[idxf.key])
            P.add('dve', lambda e, idxi=idxi: e.tensor_copy(idxi[:], idxf[:]), r=[idxf.key], w=[idxi.key])
            P.add('dve', lambda e, ig=ig, gate=gate: e.tensor_tensor(gate[:], ig[:, :, 2], ig[:, :, 3], ALU.add), r=[idxg.key], w=[gate.key])
            if dbg_idx is not None:
                dma('sp', dbg_idx[:, 4 * ex:4 * ex + 4], idxf[:], [idxf.key], [('dbg_idx', ex)])
            for j in range(4):
                P.add('pool', lambda e, xs=xs, idxi=idxi, j=j: e.indirect_dma_start(out=xs[:, j, :], out_offset=None, in_=h2_d[:, :],
                                                                                     in_offset=bass.IndirectOffsetOnAxis(ap=idxi[:, j:j + 1], axis=0)),
                      r=[idxi.key] + h2k, w=[(xs.key, j)], dma=True)
            for j in range(4):
                for k4 in range(2):
                    ps = ps_next()
                    psb = ps[:].bitcast(BF16)
                    for kk in range(4):
                        k = 4 * k4 + kk
                        P.add('pe', lambda e, psb=psb, xs=xs, j=j, k=k, kk=kk: e.transpose(psb[:, 128 * kk:128 * kk + 128], xs[:, j, 128 * k:128 * k + 128], ident_bf[:]),
                              r=[(xs.key, j), ident_bf.key], w=[ps.key])
                    evac(xsT[:, 4 * k4:4 * k4 + 4, 128 * j:128 * j + 128], psb[:, 0:512].rearrange("p (k t) -> p k t", k=4), [ps.key], [(xsT.key, j, k4)])
            xk = [(xsT.key, j, k4) for j in range(4) for k4 in range(2)]
            for s4 in range(4):
                wg, wgv = load_piece(W['w_gate'][l, ex][:, 512 * s4:512 * s4 + 512].rearrange("(k p) f -> p k f", p=128), "p (k f) -> p k f", k=8)
                wu, wuv = load_piece(W['w_up'][l, ex][:, 512 * s4:512 * s4 + 512].rearrange("(k p) f -> p k f", p=128), "p (k f) -> p k f", k=8)
                for f4 in range(4):
                    fc = 4 * s4 + f4
                    psA, psB = ps_next(), ps_next()
                    sg = sgs[fc % 2]
                    for k in range(8):
                        P.add('pe', lambda e, psA=psA, wgv=wgv, k=k, f4=f4: e.matmul(psA[:], wgv[:, k, 128 * f4:128 * f4 + 128], xsT[:, k, :], start=(k == 0), stop=(k == 7)),
                              r=[wg.key] + (xk if k == 0 else []), w=[psA.key])
                    for k in range(8):
                        P.add('pe', lambda e, psB=psB, wuv=wuv, k=k, f4=f4: e.matmul(psB[:], wuv[:, k, 128 * f4:128 * f4 + 128], xsT[:, k, :], start=(k == 0), stop=(k == 7)),
                              r=[wu.key] + (xk if k == 0 else []), w=[psB.key])
                    P.add('act', lambda e, psA=psA, sg=sg: e.activation(sg[:], psA[:], AF.Silu), r=[psA.key], w=[sg.key])
                    P.add('dve', lambda e, psB=psB, sg=sg, fc=fc: e.tensor_tensor(gT[:, fc, :], psB[:], sg[:], ALU.mult), r=[psB.key, sg.key], w=[(gT.key, fc)])
            wds = []
            for s4 in range(4):
                wd, wdv = load_piece(W['w_down'][l, ex][512 * s4:512 * s4 + 512, :].rearrange("(k p) d -> p k d", p=128), "p (k d) -> p k d", k=4)
                wds.append((wd, wdv))
            gk = [(gT.key, fc) for fc in range(16)]
            for j in range(4):
                yo = yos[j % 2]
                for nh in range(2):
                    ps = ps_next()
                    for fc in range(16):
                        wd, wdv = wds[fc // 4]
                        P.add('pe', lambda e, ps=ps, fc=fc, j=j, nh=nh, wdv=wdv: e.matmul(ps[:], gT[:, fc, 128 * j:128 * j + 128], wdv[:, fc % 4, 512 * nh:512 * nh + 512], start=(fc == 0), stop=(fc == 15)),
                              r=[wd.key] + (gk if fc == 0 else []), w=[ps.key])
                    P.add('act', lambda e, ps=ps, yo=yo, nh=nh, gate=gate, j=j: e.activation(yo[:, 512 * nh:512 * nh + 512], ps[:], AF.Copy, scale=gate[:, j:j + 1]),
                          r=[ps.key, gate.key], w=[(yo.key, nh)])
                P.add('pool', lambda e, yo=yo, idxi=idxi, j=j: e.indirect_dma_start(out=xr[:, :], out_offset=bass.IndirectOffsetOnAxis(ap=idxi[:, j:j + 1], axis=0),
                                                                                     in_=yo[:, :], in_offset=None, compute_op=ALU.add),
                      r=[idxi.key, (yo.key, 0), (yo.key, 1)] + [('xr', t) for t in range(32)], w=['xr_scatter'], dma=True)
        release(m0)

    def stage_F():
        m0 = mark()
        gfb = sb([128, D], F32, 'gfb')
        dma('sp', gfb[:], bcast_rows(W['final_g'][0:1, :], 128, D), [], [gfb.key])
        xts = [sb([128, D], F32, 'xt') for _ in range(2)]
        ots = [sb([128, D], F32, 'ot') for _ in range(2)]
        junk = sb([128, D], F32, 'junk')
        sss = [sb([128, 1], F32, 'ss') for _ in range(2)]
        okeys = []
        for t in range(32):
            xt, ot, ss = xts[t % 2], ots[t % 2], sss[t % 2]
            dma('sp', xt[:], xr[128 * t:128 * t + 128, :], [('xr', t), 'xr_scatter'], [xt.key])
            P.add('act', lambda e, xt=xt, ss=ss: e.activation(junk[:], xt[:], AF.Square, accum_out=ss[:]), r=[xt.key], w=[junk.key, ss.key])
            rstd_from_ss(ss, D)
            P.add('dve', lambda e, xt=xt, ss=ss, ot=ot: e.scalar_tensor_tensor(ot[:], xt[:], ss[:, 0:1], gfb[:], ALU.mult, ALU.mult), r=[xt.key, ss.key, gfb.key], w=[ot.key])
            dma('sp', out[128 * t:128 * t + 128, :], ot[:], [ot.key], [('out', t)])
            okeys.append(('out', t))
        P.add('sp', lambda e: e.nop(), r=okeys + [k for k in dbg_keys])
        release(m0)

    dbg_keys = []
    stages = dbg.get('stages', 'AHNPEM')
    for l in range(n_layers):
        if 'A' in stages:
            stage_A(l)
        if 'H' in stages:
            stage_H(l)
        if 'N' in stages:
            stage_N(l)
        if 'P' in stages:
            stage_P(l)
        if 'E' in stages or 'M' in stages:
            mm = mark()
            state['aff_tok'] = sb([128, 32, 16], F32, 'aff_tok')
            state['affT'] = sb([16, L], F32, 'affT')
            if 'E' in stages:
                stage_E(l)
            if 'M' in stages:
                stage_M(l)
            release(mm)
        if l > 0 or True:
            for t in range(32):
                pass
    if dbg.get('final', True):
        stage_F()
    else:
        allk = set()
        for op in P.ops:
            if op[0] != 'BAR' and op[4]:
                allk.update(op[3])
        P.add('sp', lambda e: e.nop(), r=list(allk))
    nops = P.emit()
    return nc, nops


_CACHE = {}


def kernel(**inputs):
    consts = build_consts()
    nabv = build_na_bias(np.asarray(inputs['na_rpb'], np.float32))
    if 'nc' not in _CACHE:
        _CACHE['nc'] = build_program()[0]
    nc = _CACHE['nc']
    shared = {}
    for k, v in inputs.items():
        if k in ('x', 'na_rpb'):
            continue
        a = np.ascontiguousarray(np.asarray(v, np.float32))
        if k == 'final_g':
            a = a.reshape(1, D)
        shared[k] = a
    shared['nab'] = nabv
    shared.update(consts)
    x = np.asarray(inputs['x'], np.float32)
    in_maps = []
    for c in range(8):
        m = dict(shared)
        m['x'] = np.ascontiguousarray(x[c])
        in_maps.append(m)
    res = run_bass_kernel_spmd(nc, in_maps, core_ids=list(range(8)))
    return np.stack([res.results[c]['out'] for c in range(8)], axis=0).astype(np.float32)
```

```python
import math
import numpy as np
import ml_dtypes
from contextlib import ExitStack
import concourse.bass as bass
import concourse.mybir as mybir
from concourse.bass_utils import run_bass_kernel_spmd

F32 = mybir.dt.float32
BF16 = mybir.dt.bfloat16
I32 = mybir.dt.int32
AF = mybir.ActivationFunctionType
ALU = mybir.AluOpType
NPBF = ml_dtypes.bfloat16

L = 4096
D = 1024
NT = 32
DEPTH = 4
EPS = 1e-6
MAGIC = 12582912.0
NEG = -30000.0


class T:
    def __init__(self, t, key):
        self.t = t
        self.key = key

    def __getitem__(self, k):
        return self.t[k]


class Prog:
    ENG = ('sp', 'act', 'dve', 'pool', 'pe')

    def __init__(self, nc):
        self.nc = nc
        self.ops = []
        self.n_dma_sems = {'sp': 28, 'act': 4, 'pool': 28}

    def add(self, eng, fn, r=(), w=(), dma=False):
        self.ops.append((eng, fn, tuple(r), tuple(w), dma, ()))

    def barrier(self):
        self.ops.append(('BAR',))

    def _expand(self):
        new = []
        last_eng = {}
        dmas = []
        for op in self.ops:
            if op[0] == 'BAR':
                dep = set(last_eng.values()) | set(dmas)
                dmas = []
                for e in self.ENG:
                    new.append((e, (lambda en: en.nop()), (), (), False, tuple(dep)))
                    last_eng[e] = len(new) - 1
            else:
                new.append(op)
                last_eng[op[0]] = len(new) - 1
                if op[4]:
                    dmas.append(len(new) - 1)
        self.ops = new

    def emit(self):
        nc = self.nc
        self._expand()
        ops = self.ops
        n = len(ops)
        last_w = {}
        readers = {}
        deps = [None] * n
        for i, (eng, fn, r, w, dma, xd) in enumerate(ops):
            d = set(xd)
            for k in r:
                if k in last_w:
                    d.add(last_w[k])
            for k in w:
                if k in last_w:
                    d.add(last_w[k])
                for j in readers.get(k, ()):
                    d.add(j)
            d.discard(i)
            deps[i] = d
            for k in w:
                last_w[k] = i
                readers[k] = []
            for k in r:
                readers.setdefault(k, []).append(i)
        need_sig = [False] * n
        real_deps = [None] * n
        for i in range(n):
            eng_i, dma_i = ops[i][0], ops[i][4]
            rd = []
            for j in deps[i]:
                eng_j, dma_j = ops[j][0], ops[j][4]
                if dma_j or dma_i or eng_j != eng_i or eng_i != 'pe':
                    rd.append(j)
                    need_sig[j] = True
            real_deps[i] = rd
        with ExitStack() as st:
            csem = {e: st.enter_context(nc.semaphore('c_' + e)) for e in self.ENG}
            dsem = {e: [st.enter_context(nc.semaphore('d_%s%d' % (e, k))) for k in range(c)]
                    for e, c in self.n_dma_sems.items()}
            sig = [None] * n
            ccount = {e: 0 for e in csem}
            dcount = {e: [0] * len(v) for e, v in dsem.items()}
            drr = {e: 0 for e in dsem}
            prev_on_sem = [None] * n
            for i, (eng, fn, r, w, dma, xd) in enumerate(ops):
                if dma:
                    k = drr[eng]
                    drr[eng] = (k + 1) % len(dsem[eng])
                    if dcount[eng][k] > 0:
                        prev_on_sem[i] = (dsem[eng][k], dcount[eng][k])
                    dcount[eng][k] += 16
                    sig[i] = (dsem[eng][k], dcount[eng][k])
                elif need_sig[i]:
                    ccount[eng] += 1
                    sig[i] = (csem[eng], ccount[eng])
            streams = {e: [i for i in range(n) if ops[i][0] == e] for e in self.ENG}
            waited = {e: {} for e in self.ENG}

            def run(ename, e):
                for i in streams[ename]:
                    fn, dma = ops[i][1], ops[i][4]
                    ws = []
                    if prev_on_sem[i] is not None:
                        ws.append(prev_on_sem[i])
                    for j in real_deps[i]:
                        ws.append(sig[j])
                    best = {}
                    for s, v in ws:
                        if v > best.get(s.num, (None, 0))[1]:
                            best[s.num] = (s, v)
                    for s, v in best.values():
                        if waited[ename].get(s.num, 0) >= v:
                            continue
                        waited[ename][s.num] = v
                        e.wait_ge(s, v)
                    ins = fn(e)
                    if sig[i] is not None:
                        ins.then_inc(sig[i][0], 16 if dma else 1)

            with nc.Block() as block:
                @block.sync
                def _(e):
                    run('sp', e)

                @block.scalar
                def _(e):
                    run('act', e)

                @block.vector
                def _(e):
                    run('dve', e)

                @block.gpsimd
                def _(e):
                    run('pool', e)

                @block.tensor
                def _(e):
                    run('pe', e)
        return n


def _bf(a):
    return np.ascontiguousarray(np.asarray(a, np.float32).astype(NPBF))


def na_windows():
    rs = np.clip(np.arange(64) - 4, 0, 56)
    win = []
    for j in range(32):
        rows = [r for r in range(64) if rs[r] <= 2 * j + 1 and rs[r] + 7 >= 2 * j]
        rlo = min(rows) // 2 * 2
        rhi = max(rows) // 2 * 2 + 1
        win.append((rlo, rhi))
    return win


def na_type(j):
    return j if j < 4 else (4 if j <= 27 else 5 + (j - 28))


NA_TYPE_REP = [0, 1, 2, 3, 10, 28, 29, 30, 31]


def build_na_bias(rpb):
    win = na_windows()
    rs = np.clip(np.arange(64) - 4, 0, 56)
    cs = np.clip(np.arange(64) - 8, 0, 48)
    out = np.full((DEPTH, 8, 128, 9, 768), NEG, np.float32)
    kc = np.arange(64)
    c = np.arange(64)
    inwin_c = (kc[:, None] >= cs[None, :]) & (kc[:, None] < cs[None, :] + 16)
    dc = np.clip(kc[:, None] - c[None, :] + 15, 0, 30)
    for ti, j in enumerate(NA_TYPE_REP):
        rlo, rhi = win[j]
        r = np.arange(rlo, rhi + 1)
        kr = 2 * j + np.arange(2)
        inwin_r = (kr[:, None] >= rs[r][None, :]) & (kr[:, None] < rs[r][None, :] + 8)
        dr = np.clip(kr[:, None] - r[None, :] + 7, 0, 14)
        m = inwin_r[:, None, :, None] & inwin_c[None, :, None, :]
        tab = rpb[:, :, dr[:, None, :, None], dc[None, :, None, :]]
        tab = np.where(m[None, None], tab, NEG).reshape(DEPTH, 8, 128, len(r) * 64)
        out[:, :, :, ti, :len(r) * 64] = tab
    return _bf(out)


def build_consts():
    c = {}
    c['ident_bf'] = _bf(np.eye(128))
    c['ident_f'] = np.eye(128, dtype=np.float32)
    p = np.concatenate([np.arange(L), np.concatenate([[0], L - np.arange(1, L)])]).astype(np.float64)
    t = p / (L - 1)
    ang = 2.0 * math.pi * p / L
    f = np.linspace(1e-4, 15.0, 16)
    zf = np.concatenate([t[None, :], np.cos(f[:, None] * ang[None, :]), -np.sin(f[:, None] * ang[None, :])], axis=0)
    c['zfT'] = np.ascontiguousarray(zf.astype(np.float32))
    deltas = np.abs(np.linspace(math.log(1e-2) / 0.3, math.log(1e-2) / 1.5, 256))
    dec = np.exp(-t[:, None] * deltas[None, :])
    dec[L, :] = 0.0
    c['decayF'] = np.ascontiguousarray(dec.astype(np.float32))
    n1 = np.arange(128)[:, None]
    k1 = np.arange(128)[None, :]
    a1 = 2 * math.pi * n1 * k1 / 128.0
    c['c1ns1'] = _bf(np.stack([np.cos(a1), -np.sin(a1)], axis=1))
    a2 = 2 * math.pi * np.arange(128)[:, None] * np.arange(64)[None, :] / 128.0
    c['cbsb'] = _bf(np.stack([np.cos(a2), -np.sin(a2)], axis=1) / 8192.0)
    n2 = np.arange(64)
    k2 = np.arange(64)
    gall = np.zeros((128, 128, 3, 128), np.float64)
    for kk in range(128):
        th = 2 * math.pi * (n2[:, None] * k2[None, :] / 64.0 + n2[:, None] * kk / 8192.0)
        gr, gi = np.cos(th), -np.sin(th)
        gf = np.zeros((128, 128))
        gf[0:64, 0:64] = gr
        gf[64:128, 0:64] = -gi
        gf[0:64, 64:128] = gi
        gf[64:128, 64:128] = gr
        gall[:, kk, 0, :] = gf
        gall[:, kk, 1, :] = np.concatenate([gf[:, 64:128], gf[:, 0:64]], axis=1)
        ph = th.T
        ir, ii = np.cos(ph), np.sin(ph)
        gfi = np.zeros((128, 128))
        gfi[0:64, 0:64] = ir
        gfi[64:128, 0:64] = -ii
        gfi[0:64, 64:128] = ii
        gfi[64:128, 64:128] = ir
        gall[:, kk, 2, :] = gfi
    c['gall'] = _bf(gall)
    pa = np.zeros((128, 4, 5, 128), np.float64)
    tt = np.arange(L)
    for g, w in enumerate((2, 4, 8, 16)):
        lo = np.clip(tt - w // 2, 0, L)
        hi = np.clip(tt + w // 2, 0, L)
        cnt = (hi - lo).astype(np.float64)

        def blk(tile_t, tile_s):
            tg = tile_t * 128 + np.arange(128)
            sg = tile_s * 128 + np.arange(128)
            m = ((sg[:, None] >= lo[tg][None, :]) & (sg[:, None] < hi[tg][None, :])) / cnt[tg][None, :]
            m = m - (sg[:, None] == tg[None, :])
            return m
        pa[:, g, 0, :] = blk(5, 5)
        pa[:, g, 1, :] = blk(0, 0)
        pa[:, g, 2, :] = blk(31, 31)
        pa[:, g, 3, :] = blk(5, 4)
        pa[:, g, 4, :] = blk(5, 6)
    c['poola'] = _bf(pa)
    ut = (np.arange(128)[:, None] < np.arange(128)[None, :]).astype(np.float32)
    c['ut_bf'] = _bf(ut)
    c['ones_bf'] = _bf(np.ones((128, 128)))
    tok = np.zeros((128, 32, 2), np.float32)
    tok[:, :, 0] = np.arange(32)[None, :]
    tok[:, :, 1] = np.arange(128)[:, None]
    c['tokc'] = _bf(tok)
    c['iota512'] = np.ascontiguousarray(np.broadcast_to(np.arange(512, dtype=np.float32)[None, :], (128, 512)))
    return c


CONST_SPECS = None


def build_program(n_layers=DEPTH, dbg=None):
    nc = bass.Bass("TRN2", target_bir_lowering=False)
    P = Prog(nc)
    dbg = dbg or {}

    def din(name, shape, dt=F32):
        return nc.dram_tensor(name, list(shape), dt, kind="ExternalInput").ap()

    def dscr(name, shape, dt=F32):
        kind = "ExternalOutput" if name in dbg else "Internal"
        return nc.dram_tensor(name, list(shape), dt, kind=kind).ap()

    x_in = din('x', [L, D])
    W = {}
    for name, shape in [('norm1_g', [DEPTH, D]), ('w_in', [DEPTH, D, 2560]), ('hy_short_w', [DEPTH, 3, 768]),
                        ('hy_short_b', [DEPTH, 768]), ('hy_f_w1', [DEPTH, 33, 64]), ('hy_f_b1', [DEPTH, 64]),
                        ('hy_f_w2', [DEPTH, 64, 64]), ('hy_f_b2', [DEPTH, 64]), ('hy_f_wout', [DEPTH, 64, 512]),
                        ('hy_f_freq', [DEPTH, 64]), ('hy_skip', [DEPTH, 256]), ('pool_w', [DEPTH, 4, 64, 64]),
                        ('pool_scale', [DEPTH, 256]), ('mix_norm_g', [DEPTH, D]), ('w_out', [DEPTH, D, D]),
                        ('norm2_g', [DEPTH, D]), ('w_router', [DEPTH, D, 16]), ('w_gate', [DEPTH, 16, D, 2048]),
                        ('w_up', [DEPTH, 16, D, 2048]), ('w_down', [DEPTH, 16, 2048, D]), ('final_g', [1, D])]:
        W[name] = din(name, shape)
    nab = din('nab', [DEPTH, 8, 128, 9, 768], BF16)
    C = {}
    for name, shape, dt in [('ident_bf', [128, 128], BF16), ('ident_f', [128, 128], F32), ('zfT', [33, 8192], F32),
                            ('decayF', [8192, 256], F32), ('c1ns1', [128, 2, 128], BF16), ('cbsb', [128, 2, 64], BF16),
                            ('gall', [128, 128, 3, 128], BF16), ('poola', [128, 4, 5, 128], BF16),
                            ('ut_bf', [128, 128], BF16), ('ones_bf', [128, 128], BF16), ('tokc', [128, 32, 2], BF16),
                            ('iota512', [128, 512], F32)]:
        C[name] = din(name, shape, dt)
    out = nc.dram_tensor('out', [L, D], F32, kind="ExternalOutput").ap()

    xr = dscr('xr', [L, D])
    hyT_d = dscr('hyT_d', [768, L], BF16)
    qT_d = dscr('qT_d', [512, L], BF16)
    kT_d = dscr('kT_d', [512, L], BF16)
    v_d = dscr('v_d', [L, 520], BF16)
    pool_d = dscr('pool_d', [L, 256], BF16)
    z_d = dscr('z_d', [L, 256], BF16)
    x0_d = dscr('x0_d', [L, 256], BF16)
    filt_d = dscr('filt_d', [8192, 256], BF16)
    Yz_d = dscr('Yz_d', [2, 128, 64, 256], BF16)
    Yf_d = dscr('Yf_d', [2, 128, 64, 256], BF16)
    U_d = dscr('U_d', [2, 64, 128, 256], BF16)
    ymix_d = dscr('ymix_d', [L, D])
    h2_d = dscr('h2_d', [L, D], BF16)
    dbg_aff = dscr('dbg_aff', [128, 512]) if 'dbg_aff' in dbg else None
    dbg_idx = dscr('dbg_idx', [128, 64]) if 'dbg_idx' in dbg else None
    dbg_affT = dscr('dbg_affT', [16, L]) if 'dbg_affT' in dbg else None
    dbg_lo = dscr('dbg_lo', [16, 4]) if 'dbg_lo' in dbg else None
    dbg_mask = dscr('dbg_mask', [128, 512]) if 'dbg_mask' in dbg else None
    dbg_rh = dscr('dbg_rh', [128, 2048], BF16) if 'dbg_rh' in dbg else None
    dbg_sel = dscr('dbg_sel', [128, 512], BF16) if 'dbg_sel' in dbg else None
    dbg_idxg = dscr('dbg_idxg', [128, 16]) if 'dbg_idxg' in dbg else None
    dbg_maskT = dscr('dbg_maskT', [16, L]) if 'dbg_maskT' in dbg else None
    dbg_afftok = dscr('dbg_afftok', [128, 512]) if 'dbg_afftok' in dbg else None

    SB_BASE = 16640
    SB_END = 212000
    sb_off = [SB_BASE]
    uid = [0]

    def sb(shape, dt, name=None):
        uid[0] += 1
        nm = '%s_%d' % (name or 't', uid[0])
        esz = 4 if dt in (F32, I32) else 2
        sz = int(np.prod(shape[1:])) * esz
        sz = (sz + 63) // 64 * 64
        o = sb_off[0]
        assert o + sz <= SB_END, ('SBUF overflow', nm, o, sz)
        sb_off[0] = o + sz
        return T(nc.alloc_sbuf_tensor_at(nm, list(shape), dt, offset=o), nm)

    def mark():
        return sb_off[0]

    def release(m):
        sb_off[0] = m
        P.barrier()

    PS = []
    for i in range(8):
        PS.append(T(nc.alloc_psum_tensor('ps%d' % i, [128, 512], F32), 'ps%d' % i))
    ps_rr = [0]

    def ps_next():
        t = PS[ps_rr[0] % 8]
        ps_rr[0] += 1
        return t

    def dma(q, out_ap, in_ap, r, w, **kw):
        P.add(q, lambda e: e.dma_start(out=out_ap, in_=in_ap, **kw), r=r, w=w, dma=True)

    ident_bf = sb([128, 128], BF16, 'identbf')
    ident_f = sb([128, 128], F32, 'identf')
    dma('sp', ident_bf[:], C['ident_bf'], [], [ident_bf.key])
    dma('sp', ident_f[:], C['ident_f'], [], [ident_f.key])
    eps_t = sb([128, 1], F32, 'eps')
    P.add('dve', lambda e: e.memset(eps_t[:], EPS), w=[eps_t.key])
    persist_mark = mark()

    def bcast_rows(src_row_ap, n_part, width):
        return src_row_ap.broadcast_to([n_part, width])

    def rstd_from_ss(ss, n):
        P.add('act', lambda e: e.activation(ss[:], ss[:], AF.Sqrt, bias=eps_t[:], scale=1.0 / n), r=[ss.key, eps_t.key], w=[ss.key])
        P.add('dve', lambda e: e.reciprocal(ss[:], ss[:]), r=[ss.key], w=[ss.key])

    evac_rr = [0]

    def evac(out_ap, in_ap, r, w, scale=None):
        evac_rr[0] += 1
        if scale is not None:
            P.add('act', lambda e: e.mul(out_ap, in_ap, scale), r=r, w=w)
        elif evac_rr[0] % 2 == 0:
            P.add('act', lambda e: e.copy(out_ap, in_ap), r=r, w=w)
        else:
            P.add('dve', lambda e: e.tensor_copy(out_ap, in_ap), r=r, w=w)

    def stage_A(l):
        m0 = mark()
        xsrc = x_in if l == 0 else xr
        win = sb([128, 8, 2560], BF16, 'win')
        wv = W['w_in'][l].rearrange("(k p) n -> p k n", p=128)
        for k in range(8):
            dma('pool', win[:, k, :], wv[:, k, :], [], [(win.key, k)])
        g1b = sb([128, D], F32, 'g1b')
        dma('sp', g1b[:], bcast_rows(W['norm1_g'][l:l + 1, :], 128, D), [], [g1b.key])
        xts = [sb([128, D], F32, 'xt') for _ in range(2)]
        junk = sb([128, D], F32, 'junk')
        sss = [sb([128, 1], F32, 'ss') for _ in range(2)]
        hbs = [sb([128, D], BF16, 'hb') for _ in range(2)]
        hTs = [sb([128, 8, 512], BF16, 'hT') for _ in range(2)]
        fms = [sb([128, 14, 512], BF16, 'fm') for _ in range(2)]
        vts = [sb([128, 8, 65], BF16, 'vt') for _ in range(2)]
        pts = [sb([128, 256], BF16, 'pt') for _ in range(2)]
        for vt in vts:
            P.add('pool', lambda e, vt=vt: e.memset(vt[:], 1.0), w=[vt.key])
        tcount = 0
        for st in range(8):
            hT = hTs[st % 2]
            fm = fms[st % 2]
            for a in range(4):
                t = 4 * st + a
                xt = xts[tcount % 2]
                ss = sss[tcount % 2]
                hb = hbs[tcount % 2]
                tcount += 1
                dma('sp', xt[:], xsrc[128 * t:128 * t + 128, :], [('xr', t), 'xr_scatter'], [xt.key])
                P.add('act', lambda e, xt=xt, ss=ss: e.activation(junk[:], xt[:], AF.Square, accum_out=ss[:]),
                      r=[xt.key], w=[junk.key, ss.key])
                rstd_from_ss(ss, D)
                P.add('dve', lambda e, xt=xt, ss=ss, hb=hb: e.scalar_tensor_tensor(hb[:], xt[:], ss[:, 0:1], g1b[:], ALU.mult, ALU.mult),
                      r=[xt.key, ss.key, g1b.key], w=[hb.key])
                for k4 in range(2):
                    ps = ps_next()
                    psb = ps[:].bitcast(BF16)
                    for kk in range(4):
                        k = 4 * k4 + kk
                        P.add('pe', lambda e, psb=psb, hb=hb, k=k, kk=kk: e.transpose(psb[:, 128 * kk:128 * kk + 128], hb[:, 128 * k:128 * k + 128], ident_bf[:]),
                              r=[hb.key, ident_bf.key], w=[ps.key])
                    evac(hT[:, 4 * k4:4 * k4 + 4, 128 * a:128 * a + 128], psb[:, 0:512].rearrange("p (k t) -> p k t", k=4),
                         [ps.key], [(hT.key, a)])
            hkeys = [(hT.key, a) for a in range(4)]
            for cc in range(14):
                ps = ps_next()
                col = 128 * cc
                for k in range(8):
                    P.add('pe', lambda e, ps=ps, k=k, col=col, hT=hT: e.matmul(ps[:], win[:, k, col:col + 128], hT[:, k, :], start=(k == 0), stop=(k == 7)),
                          r=hkeys + [(win.key, k)], w=[ps.key])
                evac(fm[:, cc, :], ps[:], [ps.key], [(fm.key, cc)], scale=(0.125 if 6 <= cc < 10 else None))
            ts = slice(512 * st, 512 * st + 512)
            dma('sp', hyT_d.rearrange("(c p) t -> p c t", p=128)[:, :, ts], fm[:, 0:6, :], [(fm.key, c) for c in range(6)], [('hyT', st)])
            dma('sp', qT_d.rearrange("(c p) t -> p c t", p=128)[:, :, ts], fm[:, 6:10, :], [(fm.key, c) for c in range(6, 10)], [('qT', st)])
            dma('sp', kT_d.rearrange("(c p) t -> p c t", p=128)[:, :, ts], fm[:, 10:14, :], [(fm.key, c) for c in range(10, 14)], [('kT', st)])
            for a in range(4):
                t = 4 * st + a
                vt = vts[t % 2]
                pt = pts[t % 2]
                ps = ps_next()
                for k in range(8):
                    P.add('pe', lambda e, ps=ps, k=k, a=a, hT=hT: e.matmul(ps[:], hT[:, k, 128 * a:128 * a + 128], win[:, k, 1792:2304], start=(k == 0), stop=(k == 7)),
                          r=hkeys + [(win.key, k)], w=[ps.key])
                P.add('act', lambda e, ps=ps, vt=vt: e.copy(vt[:, :, 0:64], ps[:].rearrange("p (h d) -> p h d", h=8)), r=[ps.key], w=[vt.key])
                dma('sp', v_d[128 * t:128 * t + 128, :], vt[:].rearrange("p h d -> p (h d)"), [vt.key], [('v', t)])
                ps2 = ps_next()
                for k in range(8):
                    P.add('pe', lambda e, ps2=ps2, k=k, a=a, hT=hT: e.matmul(ps2[:, 0:256], hT[:, k, 128 * a:128 * a + 128], win[:, k, 2304:2560], start=(k == 0), stop=(k == 7)),
                          r=hkeys + [(win.key, k)], w=[ps2.key])
                P.add('dve', lambda e, ps2=ps2, pt=pt: e.tensor_copy(pt[:], ps2[:, 0:256]), r=[ps2.key], w=[pt.key])
                dma('sp', pool_d[128 * t:128 * t + 128, :], pt[:], [pt.key], [('pool', t)])
        release(m0)

    def stage_H(l):
        m0 = mark()
        swT = sb([128, 3, 6], F32, 'swT')
        sbT = sb([128, 6], F32, 'sbT')
        for k3 in range(3):
            dma('sp', swT[:, k3, :], W['hy_short_w'][l, k3:k3 + 1, :].rearrange("o (c p) -> p (o c)", p=128), [], [(swT.key, k3)], allow_slow_non_contiguous=True)
        dma('sp', sbT[:], W['hy_short_b'][l:l + 1, :].rearrange("o (c p) -> p (o c)", p=128), [], [sbT.key], allow_slow_non_contiguous=True)
        ztok = sb([128, 32, 256], BF16, 'ztok')
        x0tok = sb([128, 32, 256], BF16, 'x0tok')
        m1 = mark()
        hyb = [sb([128, 4098], BF16, 'hyb') for _ in range(3)]
        t1 = sb([128, L], F32, 't1')
        ua = sb([128, L], F32, 'ua')
        ub = sb([128, L], F32, 'ub')
        zT = sb([128, L], BF16, 'zT')
        x0T = sb([128, L], BF16, 'x0T')
        for hb_ in hyb:
            P.add('pool', lambda e, hb_=hb_: e.memset(hb_[:], 0.0), w=[hb_.key])
        hyview = hyT_d.rearrange("(c p) t -> p c t", p=128)

        def conv(eng, dst, src, c):
            P.add(eng, lambda e: e.tensor_scalar(t1[:] if dst is not t1 else dst[:], src[:, 0:L], swT[:, 0, c:c + 1], sbT[:, c:c + 1], ALU.mult, ALU.add),
                  r=[src.key, (swT.key, 0), sbT.key], w=[t1.key])
            P.add(eng, lambda e: e.scalar_tensor_tensor(t1[:], src[:, 1:L + 1], swT[:, 1, c:c + 1], t1[:], ALU.mult, ALU.add),
                  r=[src.key, (swT.key, 1), t1.key], w=[t1.key])
            P.add(eng, lambda e: e.scalar_tensor_tensor(dst[:], src[:, 2:L + 2], swT[:, 2, c:c + 1], t1[:], ALU.mult, ALU.add),
                  r=[src.key, (swT.key, 2), t1.key], w=[dst.key])
        for pi in range(2):
            for which in range(3):
                dma('sp', hyb[which][:, 1:L + 1], hyview[:, 2 * which + pi, :], [('hyT', s) for s in range(8)], [hyb[which].key])
            conv('dve', x0T, hyb[0], pi)
            conv('dve', ua, hyb[1], 2 + pi)
            conv('dve', ub, hyb[2], 4 + pi)
            P.add('dve', lambda e: e.tensor_tensor(zT[:], ua[:], ub[:], ALU.mult), r=[ua.key, ub.key], w=[zT.key])
            for src, dst in ((zT, ztok), (x0T, x0tok)):
                for t4 in range(8):
                    ps = ps_next()
                    psb = ps[:].bitcast(BF16)
                    for q in range(4):
                        tt = 4 * t4 + q
                        P.add('pe', lambda e, psb=psb, src=src, tt=tt, q=q: e.transpose(psb[:, 128 * q:128 * q + 128], src[:, 128 * tt:128 * tt + 128], ident_bf[:]),
                              r=[src.key, ident_bf.key], w=[ps.key])
                    evac(dst[:, 4 * t4:4 * t4 + 4, 128 * pi:128 * pi + 128], psb[:, 0:512].rearrange("p (q c) -> p q c", q=4), [ps.key], [(dst.key, pi)])
        dma('sp', z_d.rearrange("(t p) c -> p t c", p=128), ztok[:], [(ztok.key, 0), (ztok.key, 1)], ['z_d'])
        dma('sp', x0_d.rearrange("(t p) c -> p t c", p=128), x0tok[:], [(x0tok.key, 0), (x0tok.key, 1)], ['x0_d'])
        release(m1)
        w1 = sb([33, 64], F32, 'w1')
        w2 = sb([64, 64], F32, 'w2')
        wo = sb([64, 512], F32, 'wo')
        fb = sb([64, 4], F32, 'fb')
        frb = sb([64, 2], F32, 'frb')
        dma('sp', w1[:], W['hy_f_w1'][l], [], [w1.key])
        dma('sp', w2[:], W['hy_f_w2'][l], [], [w2.key])
        dma('sp', wo[:], W['hy_f_wout'][l], [], [wo.key])
        dma('sp', fb[:, 0:1], W['hy_f_b1'][l:l + 1, :].rearrange("o p -> p o"), [], [(fb.key, 0)], allow_slow_non_contiguous=True)
        dma('sp', fb[:, 1:2], W['hy_f_b2'][l:l + 1, :].rearrange("o p -> p o"), [], [(fb.key, 1)], allow_slow_non_contiguous=True)
        dma('sp', fb[:, 2:3], W['hy_f_freq'][l:l + 1, :].rearrange("o p -> p o"), [], [(fb.key, 2)], allow_slow_non_contiguous=True)
        P.add('dve', lambda e: e.tensor_scalar(frb[:], fb[:, 0:2], fb[:, 2:3], None, ALU.mult), r=[(fb.key, 0), (fb.key, 1), (fb.key, 2)], w=[frb.key])
        zfs = [sb([33, 512], F32, 'zf') for _ in range(2)]
        arg = sb([64, 512], F32, 'arg')
        red = sb([64, 512], F32, 'red')
        h1 = sb([64, 512], F32, 'h1')
        h2 = sb([64, 512], F32, 'h2')
        decs = [sb([128, 4, 256], F32, 'dec') for _ in range(2)]
        fsts = [sb([128, 4, 256], BF16, 'fst') for _ in range(2)]

        def sin_layer(ps, dst, bcol):
            P.add('dve', lambda e: e.tensor_scalar(arg[:], ps[0:64, :], fb[:, 2:3], frb[:, bcol:bcol + 1], ALU.mult, ALU.add),
                  r=[ps.key, (fb.key, 2), frb.key], w=[arg.key])
            P.add('dve', lambda e: e.tensor_scalar(red[:], arg[:], 1.0 / (2 * math.pi), MAGIC, ALU.mult, ALU.add), r=[arg.key], w=[red.key])
            P.add('dve', lambda e: e.tensor_scalar(red[:], red[:], -MAGIC, -2 * math.pi, ALU.add, ALU.mult), r=[red.key], w=[red.key])
            P.add('dve', lambda e: e.tensor_tensor(red[:], red[:], arg[:], ALU.add), r=[red.key, arg.key], w=[red.key])
            P.add('act', lambda e: e.activation(dst[:], red[:], AF.Sin), r=[red.key], w=[dst.key])
        for pc in range(16):
            zf = zfs[pc % 2]
            dec = decs[pc % 2]
            fst = fsts[pc % 2]
            dma('sp', zf[:], C['zfT'][:, 512 * pc:512 * pc + 512], [], [zf.key])
            dma('sp', dec[:], C['decayF'][512 * pc:512 * pc + 512, :].rearrange("(q p) c -> p q c", p=128), [], [dec.key])
            ps = ps_next()
            P.add('pe', lambda e, ps=ps, zf=zf: e.matmul(ps[0:64, :], w1[:], zf[:], start=True, stop=True), r=[w1.key, zf.key], w=[ps.key])
            sin_layer(ps, h1, 0)
            ps = ps_next()
            P.add('pe', lambda e, ps=ps: e.matmul(ps[0:64, :], w2[:], h1[:], start=True, stop=True), r=[w2.key, h1.key], w=[ps.key])
            sin_layer(ps, h2, 1)
            half = 0 if pc < 8 else 1
            for qq in range(2):
                ps = ps_next()
                for q2 in range(2):
                    q = 2 * qq + q2
                    P.add('pe', lambda e, ps=ps, q=q, q2=q2, half=half: e.matmul(ps[:, 256 * q2:256 * q2 + 256], h2[:, 128 * q:128 * q + 128], wo[:, 256 * half:256 * half + 256], start=True, stop=True),
                          r=[h2.key, wo.key], w=[ps.key])
                P.add('dve', lambda e, ps=ps, qq=qq, dec=dec, fst=fst: e.tensor_tensor(fst[:, 2 * qq:2 * qq + 2, :], ps[:].rearrange("p (q c) -> p q c", q=2), dec[:, 2 * qq:2 * qq + 2, :], ALU.mult),
                      r=[ps.key, dec.key], w=[(fst.key, qq)])
            dma('sp', filt_d[512 * pc:512 * pc + 512, :].rearrange("(q p) c -> p q c", p=128), fst[:], [(fst.key, 0), (fst.key, 1)], [('filt', pc)])
        release(m1)
        c1 = sb([128, 2, 128], BF16, 'c1')
        dma('sp', c1[:], C['c1ns1'], [], [c1.key])
        z_sb = sb([64, 16384], BF16, 'z_sb')
        dma('sp', z_sb[:], z_d.rearrange("(n1 n2) c -> n1 (n2 c)", n2=64), ['z_d'], [z_sb.key])
        m2 = mark()
        f_sb = sb([128, 16384], BF16, 'f_sb')
        dma('sp', f_sb[:], filt_d.rearrange("(n1 n2) c -> n1 (n2 c)", n2=64), [('filt', pc) for pc in range(16)], [f_sb.key])
        ysts = [sb([128, 2048], BF16, 'yst') for _ in range(4)]
        yc = 0
        for src, K, dst_d, nm in ((z_sb, 64, Yz_d, 'Yz'), (f_sb, 128, Yf_d, 'Yf')):
            for grp in range(8):
                for ri in range(2):
                    yst = ysts[yc % 4]
                    yc += 1
                    for c4 in range(4):
                        ch = 4 * grp + c4
                        ps = ps_next()
                        P.add('pe', lambda e, ps=ps, src=src, K=K, ri=ri, ch=ch: e.matmul(ps[:], c1[0:K, ri, :], src[0:K, 512 * ch:512 * ch + 512], start=True, stop=True),
                              r=[c1.key, src.key], w=[ps.key])
                        evac(yst[:, 512 * c4:512 * c4 + 512], ps[:], [ps.key], [(yst.key, c4)])
                    dma('sp', dst_d[ri].rearrange("k n c -> k (n c)")[:, 2048 * grp:2048 * grp + 2048], yst[:], [(yst.key, c4) for c4 in range(4)], [(nm, ri, grp)])
        release(m2)
        galls = [sb([128, 16, 3, 128], BF16, 'gall') for _ in range(2)]
        yzs = [sb([128, 16, 256], BF16, 'yzg') for _ in range(2)]
        yfs = [sb([128, 16, 256], BF16, 'yfg') for _ in range(2)]
        usts = [sb([128, 16, 256], BF16, 'ust') for _ in range(2)]
        has = [sb([128, 512], BF16, 'ha') for _ in range(2)]
        hbs_ = [sb([128, 512], BF16, 'hb_') for _ in range(2)]
        t1s = [sb([128, 512], BF16, 'p1') for _ in range(2)]
        t2s = [sb([128, 512], BF16, 'p2') for _ in range(2)]
        allY = lambda nm: [(nm, ri, g) for ri in range(2) for g in range(8)]
        for g in range(8):
            ga = galls[g % 2]
            yz = yzs[g % 2]
            yf = yfs[g % 2]
            ust = usts[g % 2]
            dma('sp', ga[:], C['gall'][:, 16 * g:16 * g + 16, :, :], [], [ga.key])
            for ri in range(2):
                dma('sp', yz[64 * ri:64 * ri + 64, :, :], Yz_d[ri, 16 * g:16 * g + 16, :, :].rearrange("k n c -> n k c"), allY('Yz'), [(yz.key, ri)])
                dma('sp', yf[64 * ri:64 * ri + 64, :, :], Yf_d[ri, 16 * g:16 * g + 16, :, :].rearrange("k n c -> n k c"), allY('Yf'), [(yf.key, ri)])
            for kp in range(8):
                ha = has[kp % 2]
                hb_ = hbs_[kp % 2]
                p1 = t1s[kp % 2]
                p2 = t2s[kp % 2]
                psF, psFs, psX, psXs, psU = ps_next(), ps_next(), ps_next(), ps_next(), ps_next()
                for (pst, which, ysrc) in ((psF, 0, yf), (psFs, 1, yf), (psX, 0, yz), (psXs, 1, yz)):
                    for kq in range(2):
                        ki = 2 * kp + kq
                        P.add('pe', lambda e, pst=pst, which=which, ysrc=ysrc, ki=ki, kq=kq, ga=ga: e.matmul(pst[:, 256 * kq:256 * kq + 256], ga[:, ki, which, :], ysrc[:, ki, :], start=True, stop=True),
                              r=[ga.key, (ysrc.key, 0), (ysrc.key, 1)], w=[pst.key])
                P.add('act', lambda e, psF=psF, ha=ha: e.copy(ha[0:64, :], psF[0:64, :]), r=[psF.key], w=[(ha.key, 0)])
                P.add('act', lambda e, psFs=psFs, ha=ha: e.copy(ha[64:128, :], psFs[64:128, :]), r=[psFs.key], w=[(ha.key, 1)])
                P.add('act', lambda e, psFs=psFs, hb_=hb_: e.mul(hb_[0:64, :], psFs[0:64, :], -1.0), r=[psFs.key], w=[(hb_.key, 0)])
                P.add('act', lambda e, psF=psF, hb_=hb_: e.copy(hb_[64:128, :], psF[64:128, :]), r=[psF.key], w=[(hb_.key, 1)])
                P.add('dve', lambda e, psX=psX, ha=ha, p1=p1: e.tensor_tensor(p1[:], psX[:], ha[:], ALU.mult), r=[psX.key, (ha.key, 0), (ha.key, 1)], w=[p1.key])
                P.add('dve', lambda e, psXs=psXs, hb_=hb_, p2=p2: e.tensor_tensor(p2[:], psXs[:], hb_[:], ALU.mult), r=[psXs.key, (hb_.key, 0), (hb_.key, 1)], w=[p2.key])
                for kq in range(2):
                    ki = 2 * kp + kq
                    P.add('pe', lambda e, psU=psU, ki=ki, kq=kq, ga=ga, p1=p1: e.matmul(psU[:, 256 * kq:256 * kq + 256], ga[:, ki, 2, :], p1[:, 256 * kq:256 * kq + 256], start=True, stop=False),
                          r=[ga.key, p1.key], w=[psU.key])
                    P.add('pe', lambda e, psU=psU, ki=ki, kq=kq, ga=ga, p2=p2: e.matmul(psU[:, 256 * kq:256 * kq + 256], ga[:, ki, 2, :], p2[:, 256 * kq:256 * kq + 256], start=False, stop=True),
                          r=[ga.key, p2.key], w=[psU.key])
                P.add('act', lambda e, psU=psU, ust=ust, kp=kp: e.copy(ust[:, 2 * kp:2 * kp + 2, :], psU[:].rearrange("p (k c) -> p k c", k=2)), r=[psU.key], w=[(ust.key, kp)])
            for ri in range(2):
                dma('sp', U_d[ri, :, 16 * g:16 * g + 16, :], ust[64 * ri:64 * ri + 64, :, :], [(ust.key, kp) for kp in range(8)], [('U', ri, g)])
        release(m2)
        cb = sb([128, 2, 64], BF16, 'cb')
        dma('sp', cb[:], C['cbsb'], [], [cb.key])
        x0_sb = sb([64, 16384], BF16, 'x0_sb')
        dma('sp', x0_sb[:], x0_d.rearrange("(n1 n2) c -> n1 (n2 c)", n2=64), ['x0_d'], [x0_sb.key])
        skb = sb([64, 256], F32, 'skb')
        dma('sp', skb[:], bcast_rows(W['hy_skip'][l:l + 1, :], 64, 256), [], [skb.key])
        ubs = [sb([128, 2, 8, 256], BF16, 'ub') for _ in range(2)]
        youts = [sb([64, 8, 256], F32, 'yout') for _ in range(2)]
        zss = [sb([64, 512], F32, 'zs') for _ in range(2)]
        ys2 = [sb([64, 512], F32, 'ys2') for _ in range(2)]
        allU = [('U', ri, g) for ri in range(2) for g in range(8)]
        ymv = ymix_d.rearrange("(n1 n2) c -> n1 n2 c", n2=64)
        for ng in range(8):
            ub_ = ubs[ng % 2]
            yout = youts[ng % 2]
            for ri in range(2):
                dma('sp', ub_[:, ri, :, :], U_d[ri, 8 * ng:8 * ng + 8, :, :].rearrange("n k c -> k n c"), allU, [(ub_.key, ri)])
            for pr in range(4):
                ps = ps_next()
                zs = zss[pr % 2]
                y2 = ys2[pr % 2]
                n2a = 8 * ng + 2 * pr
                P.add('pe', lambda e, ps=ps, ub_=ub_, pr=pr: e.matmul(ps[0:64, :], cb[:, 0, :], ub_[:, 0, 2 * pr:2 * pr + 2, :].rearrange("p n c -> p (n c)"), start=True, stop=False),
                      r=[cb.key, (ub_.key, 0)], w=[ps.key])
                P.add('pe', lambda e, ps=ps, ub_=ub_, pr=pr: e.matmul(ps[0:64, :], cb[:, 1, :], ub_[:, 1, 2 * pr:2 * pr + 2, :].rearrange("p n c -> p (n c)"), start=False, stop=True),
                      r=[cb.key, (ub_.key, 1)], w=[ps.key])
                P.add('dve', lambda e, zs=zs, n2a=n2a: e.tensor_tensor(zs[:].rearrange("p (n c) -> p n c", n=2), z_sb[:, 256 * n2a:256 * n2a + 512].rearrange("p (n c) -> p n c", n=2),
                                                                          skb[:].unsqueeze(1).broadcast_to([64, 2, 256]), ALU.mult),
                      r=[z_sb.key, skb.key], w=[zs.key])
                P.add('dve', lambda e, ps=ps, zs=zs, y2=y2: e.tensor_tensor(y2[:], ps[0:64, :], zs[:], ALU.add), r=[ps.key, zs.key], w=[y2.key])
                P.add('dve', lambda e, y2=y2, yout=yout, pr=pr, n2a=n2a: e.tensor_tensor(yout[:, 2 * pr:2 * pr + 2, :].rearrange("p n c -> p (n c)"), y2[:], x0_sb[:, 256 * n2a:256 * n2a + 512], ALU.mult),
                      r=[y2.key, x0_sb.key], w=[(yout.key, pr)])
            dma('sp', ymv[:, 8 * ng:8 * ng + 8, 0:256], yout[:], [(yout.key, pr) for pr in range(4)], [('ymix_hy', ng)])
        release(m0)

    def stage_N(l):
        m0 = mark()
        win_ = na_windows()
        vaug = sb([128, 32, 520], BF16, 'vaug')
        dma('sp', vaug[:], v_d.rearrange("(t p) c -> p t c", p=128), [('v', t) for t in range(32)], [vaug.key])
        PT = sb([128, 32, 768], BF16, 'PT')
        qcs = [sb([128, L], BF16, 'qc') for _ in range(2)]
        kcs = [sb([128, L], BF16, 'kc') for _ in range(2)]
        nbs = [sb([128, 9, 768], BF16, 'nb') for _ in range(2)]
        yns = [sb([128, 32, 64], F32, 'yn') for _ in range(2)]
        rcs = [sb([128, 1], F32, 'rc') for _ in range(4)]
        rcc = 0
        qv = qT_d.rearrange("(c p) t -> p c t", p=128)
        kv = kT_d.rearrange("(c p) t -> p c t", p=128)
        for h in range(8):
            c, s = h // 2, h % 2
            qc, kc = qcs[c % 2], kcs[c % 2]
            if s == 0:
                dma('sp', qc[:], qv[:, c, :], [('qT', st) for st in range(8)], [qc.key])
                dma('sp', kc[:], kv[:, c, :], [('kT', st) for st in range(8)], [kc.key])
            nb = nbs[h % 2]
            dma('sp', nb[:], nab[l, h], [], [nb.key])
            yn = yns[h % 2]
            pb = 64 * s
            for j in range(32):
                rlo, rhi = win_[j]
                nq = (rhi - rlo + 1) * 64
                q0 = rlo * 64
                ty = na_type(j)
                off = 0
                while off < nq:
                    n = min(512, nq - off)
                    ps = ps_next()
                    P.add('pe', lambda e, ps=ps, kc=kc, qc=qc, pb=pb, j=j, q0=q0, off=off, n=n: e.matmul(ps[:, 0:n], kc[pb:pb + 64, 128 * j:128 * j + 128], qc[pb:pb + 64, q0 + off:q0 + off + n], start=True, stop=False),
                          r=[kc.key, qc.key], w=[ps.key])
                    P.add('pe', lambda e, ps=ps, nb=nb, ty=ty, off=off, n=n: e.matmul(ps[:, 0:n], ident_bf[:], nb[:, ty, off:off + n], start=False, stop=True),
                          r=[nb.key, ident_bf.key], w=[ps.key])
                    P.add('act', lambda e, ps=ps, j=j, off=off, n=n: e.activation(PT[:, j, off:off + n], ps[:, 0:n], AF.Exp), r=[ps.key], w=[(PT.key, j)])
                    off += n
            for i in range(32):
                js = [j for j in range(32) if win_[j][0] <= 2 * i and win_[j][1] >= 2 * i + 1]
                ps = ps_next()
                for n_, j in enumerate(js):
                    o = (2 * i - win_[j][0]) * 64
                    P.add('pe', lambda e, ps=ps, j=j, o=o, i=i, h=h, n_=n_, last=(n_ == len(js) - 1): e.matmul(ps[:, 0:65], PT[:, j, o:o + 128], vaug[:, j, 65 * h:65 * h + 65], start=(n_ == 0), stop=last),
                          r=[(PT.key, j), vaug.key], w=[ps.key])
                rc = rcs[rcc % 4]
                rcc += 1
                P.add('dve', lambda e, ps=ps, rc=rc: e.reciprocal(rc[:], ps[:, 64:65]), r=[ps.key], w=[rc.key])
                P.add('dve', lambda e, ps=ps, rc=rc, yn=yn, i=i: e.tensor_scalar(yn[:, i, :], ps[:, 0:64], rc[:, 0:1], None, ALU.mult), r=[ps.key, rc.key], w=[(yn.key, i)])
            dma('sp', ymix_d.rearrange("(t p) c -> p t c", p=128)[:, :, 256 + 64 * h:256 + 64 * h + 64], yn[:], [(yn.key, i) for i in range(32)], [('ymix_na', h)])
        release(m0)

    def stage_P(l):
        m0 = mark()
        u_sb = sb([128, 32, 256], BF16, 'u_sb')
        dma('sp', u_sb[:], pool_d.rearrange("(t p) c -> p t c", p=128), [('pool', t) for t in range(32)], [u_sb.key])
        pa = sb([128, 4, 5, 128], BF16, 'pa')
        dma('sp', pa[:], C['poola'], [], [pa.key])
        pw = sb([64, 4, 64], BF16, 'pw')
        dma('pool', pw[:], W['pool_w'][l].rearrange("g c d -> c g d"), [], [pw.key])
        psb_ = sb([128, 256], F32, 'pscale')
        dma('sp', psb_[:], bcast_rows(W['pool_scale'][l:l + 1, :], 128, 256), [], [psb_.key])
        pTs = [sb([64, 4, 128], BF16, 'pT') for _ in range(2)]
        yp = sb([128, 32, 256], F32, 'yp')
        for t in range(32):
            pT = pTs[t % 2]
            ps = ps_next()
            for g in range(4):
                srcs = []
                if t > 0:
                    srcs.append((t - 1, 3))
                srcs.append((t, 1 if t == 0 else (2 if t == 31 else 0)))
                if t < 31:
                    srcs.append((t + 1, 4))
                for n_, (tp, var) in enumerate(srcs):
                    P.add('pe', lambda e, ps=ps, g=g, tp=tp, var=var, n_=n_, last=(n_ == len(srcs) - 1): e.matmul(ps[0:64, 128 * g:128 * g + 128], u_sb[:, tp, 64 * g:64 * g + 64], pa[:, g, var, :], start=(n_ == 0), stop=last),
                          r=[u_sb.key, pa.key], w=[ps.key])
            P.add('act', lambda e, ps=ps, pT=pT: e.copy(pT[:].rearrange("p g t -> p (g t)"), ps[0:64, :]), r=[ps.key], w=[pT.key])
            ps2 = ps_next()
            for g in range(4):
                P.add('pe', lambda e, ps2=ps2, g=g, pT=pT: e.matmul(ps2[:, 64 * g:64 * g + 64], pT[:, g, :], pw[:, g, :], start=True, stop=True),
                      r=[pT.key, pw.key], w=[ps2.key])
            P.add('dve', lambda e, ps2=ps2, t=t: e.tensor_tensor(yp[:, t, :], ps2[:, 0:256], psb_[:], ALU.mult), r=[ps2.key, psb_.key], w=[(yp.key, t)])
        dma('sp', ymix_d.rearrange("(t p) c -> p t c", p=128)[:, :, 768:1024], yp[:], [(yp.key, t) for t in range(32)], ['ymix_pool'])
        release(m0)

    state = {}

    def stage_E(l):
        m0 = mark()
        aff_tok, affT = state['aff_tok'], state['affT']
        xsrc = x_in if l == 0 else xr
        wo_sb = sb([128, 8, D], BF16, 'wo_sb')
        wv = W['w_out'][l].rearrange("(k p) n -> p k n", p=128)
        for k in range(8):
            dma('pool', wo_sb[:, k, :], wv[:, k, :], [], [(wo_sb.key, k)])
        wr_sb = sb([128, 8, 16], F32, 'wr_sb')
        dma('sp', wr_sb[:], W['w_router'][l].rearrange("(k p) n -> p k n", p=128), [], [wr_sb.key])
        gmb = sb([128, D], F32, 'gmb')
        g2b = sb([128, D], F32, 'g2b')
        dma('sp', gmb[:], bcast_rows(W['mix_norm_g'][l:l + 1, :], 128, D), [], [gmb.key])
        dma('sp', g2b[:], bcast_rows(W['norm2_g'][l:l + 1, :], 128, D), [], [g2b.key])
        yms = [sb([128, D], F32, 'ym') for _ in range(2)]
        xts = [sb([128, D], F32, 'xt') for _ in range(2)]
        junk = sb([128, D], F32, 'junk')
        ss3 = [sb([128, 4], F32, 'ss3') for _ in range(2)]
        mxs = [sb([128, D], BF16, 'mx') for _ in range(2)]
        mTs = [sb([128, 8, 128], BF16, 'mT') for _ in range(2)]
        x1s = [sb([128, D], F32, 'x1') for _ in range(2)]
        h2fs = [sb([128, D], F32, 'h2f') for _ in range(2)]
        h2bs = [sb([128, D], BF16, 'h2b') for _ in range(2)]
        h2Ts = [sb([128, 8, 128], F32, 'h2T') for _ in range(2)]
        lgs = [sb([128, 16], F32, 'lg') for _ in range(2)]
        sm4 = [sb([128, 4], F32, 'sm4') for _ in range(2)]
        segs = [(0, 256), (256, 768), (768, 1024)]
        ymkeys = [('ymix_hy', ng) for ng in range(8)] + [('ymix_na', h) for h in range(8)] + ['ymix_pool']
        for t in range(32):
            b = t % 2
            ym, xt, s3, mx, mT, x1, h2f, h2b, h2T, lg, sm = yms[b], xts[b], ss3[b], mxs[b], mTs[b], x1s[b], h2fs[b], h2bs[b], h2Ts[b], lgs[b], sm4[b]
            rows = slice(128 * t, 128 * t + 128)
            dma('sp', ym[:], ymix_d[rows, :], ymkeys, [ym.key])
            dma('sp', xt[:], xsrc[rows, :], [('xr', t), 'xr_scatter'], [xt.key])
            for si, (a0, a1) in enumerate(segs):
                P.add('act', lambda e, ym=ym, s3=s3, si=si, a0=a0, a1=a1: e.activation(junk[:, a0:a1], ym[:, a0:a1], AF.Square, accum_out=s3[:, si:si + 1]),
                      r=[ym.key], w=[junk.key, (s3.key, si)])
                P.add('act', lambda e, s3=s3, si=si, n=a1 - a0: e.activation(s3[:, si:si + 1], s3[:, si:si + 1], AF.Sqrt, bias=eps_t[:], scale=1.0 / n), r=[(s3.key, si), eps_t.key], w=[(s3.key, si)])
                P.add('dve', lambda e, s3=s3, si=si: e.reciprocal(s3[:, si:si + 1], s3[:, si:si + 1]), r=[(s3.key, si)], w=[(s3.key, si)])
                P.add('dve', lambda e, ym=ym, s3=s3, mx=mx, si=si, a0=a0, a1=a1: e.scalar_tensor_tensor(mx[:, a0:a1], ym[:, a0:a1], s3[:, si:si + 1], gmb[:, a0:a1], ALU.mult, ALU.mult),
                      r=[ym.key, (s3.key, si), gmb.key], w=[(mx.key, si)])
            mxk = [(mx.key, si) for si in range(3)]
            for k4 in range(2):
                ps = ps_next()
                psb = ps[:].bitcast(BF16)
                for kk in range(4):
                    k = 4 * k4 + kk
                    P.add('pe', lambda e, psb=psb, mx=mx, k=k, kk=kk: e.transpose(psb[:, 128 * kk:128 * kk + 128], mx[:, 128 * k:128 * k + 128], ident_bf[:]),
                          r=mxk + [ident_bf.key], w=[ps.key])
                evac(mT[:, 4 * k4:4 * k4 + 4, :], psb[:, 0:512].rearrange("p (k t) -> p k t", k=4), [ps.key], [(mT.key, k4)])
            for nh in range(2):
                ps = ps_next()
                for k in range(8):
                    P.add('pe', lambda e, ps=ps, k=k, nh=nh, mT=mT: e.matmul(ps[:], mT[:, k, :], wo_sb[:, k, 512 * nh:512 * nh + 512], start=(k == 0), stop=(k == 7)),
                          r=[(mT.key, 0), (mT.key, 1), (wo_sb.key, k)], w=[ps.key])
                P.add('dve', lambda e, ps=ps, nh=nh, xt=xt, x1=x1: e.tensor_tensor(x1[:, 512 * nh:512 * nh + 512], ps[:], xt[:, 512 * nh:512 * nh + 512], ALU.add),
                      r=[ps.key, xt.key], w=[(x1.key, nh)])
            x1k = [(x1.key, 0), (x1.key, 1)]
            dma('sp', xr[rows, :], x1[:], x1k, [('xr', t)])
            P.add('act', lambda e, x1=x1, s3=s3: e.activation(junk[:], x1[:], AF.Square, accum_out=s3[:, 3:4]), r=x1k, w=[junk.key, (s3.key, 3)])
            P.add('act', lambda e, s3=s3: e.activation(s3[:, 3:4], s3[:, 3:4], AF.Sqrt, bias=eps_t[:], scale=1.0 / D), r=[(s3.key, 3), eps_t.key], w=[(s3.key, 3)])
            P.add('dve', lambda e, s3=s3: e.reciprocal(s3[:, 3:4], s3[:, 3:4]), r=[(s3.key, 3)], w=[(s3.key, 3)])
            P.add('dve', lambda e, x1=x1, s3=s3, h2f=h2f: e.scalar_tensor_tensor(h2f[:], x1[:], s3[:, 3:4], g2b[:], ALU.mult, ALU.mult), r=x1k + [(s3.key, 3), g2b.key], w=[h2f.key])
            P.add('act', lambda e, h2f=h2f, h2b=h2b: e.copy(h2b[:], h2f[:]), r=[h2f.key], w=[h2b.key])
            dma('sp', h2_d[rows, :], h2b[:], [h2b.key], [('h2', t)])
            for k4 in range(2):
                ps = ps_next()
                for kk in range(4):
                    k = 4 * k4 + kk
                    P.add('pe', lambda e, ps=ps, h2f=h2f, k=k, kk=kk: e.transpose(ps[:, 128 * kk:128 * kk + 128], h2f[:, 128 * k:128 * k + 128], ident_f[:]),
                          r=[h2f.key, ident_f.key], w=[ps.key])
                evac(h2T[:, 4 * k4:4 * k4 + 4, :], ps[:].rearrange("p (k t) -> p k t", k=4), [ps.key], [(h2T.key, k4)])
            ps = ps_next()
            for k in range(8):
                P.add('pe', lambda e, ps=ps, k=k, h2T=h2T: e.matmul(ps[:, 0:16], h2T[:, k, :], wr_sb[:, k, :], start=(k == 0), stop=(k == 7)),
                      r=[(h2T.key, 0), (h2T.key, 1), wr_sb.key], w=[ps.key])
            P.add('dve', lambda e, ps=ps, lg=lg: e.tensor_copy(lg[:], ps[:, 0:16]), r=[ps.key], w=[lg.key])
            P.add('dve', lambda e, lg=lg, sm=sm: e.reduce_max(sm[:, 0:1], lg[:], mybir.AxisListType.X), r=[lg.key], w=[(sm.key, 0)])
            P.add('dve', lambda e, sm=sm: e.tensor_scalar(sm[:, 1:2], sm[:, 0:1], -1.0, None, ALU.mult), r=[(sm.key, 0)], w=[(sm.key, 1)])
            P.add('act', lambda e, lg=lg, sm=sm: e.activation(lg[:], lg[:], AF.Exp, bias=sm[:, 1:2], accum_out=sm[:, 2:3]), r=[lg.key, (sm.key, 1)], w=[lg.key, (sm.key, 2)])
            P.add('dve', lambda e, sm=sm: e.reciprocal(sm[:, 3:4], sm[:, 2:3]), r=[(sm.key, 2)], w=[(sm.key, 3)])
            P.add('dve', lambda e, lg=lg, sm=sm, t=t: e.tensor_scalar(aff_tok[:, t, :], lg[:], sm[:, 3:4], None, ALU.mult), r=[lg.key, (sm.key, 3)], w=[(aff_tok.key, t)])
            ps = ps_next()
            P.add('pe', lambda e, ps=ps, t=t: e.transpose(ps[0:16, 0:128], aff_tok[:, t, :], ident_f[:]), r=[(aff_tok.key, t), ident_f.key], w=[ps.key])
            P.add('act', lambda e, ps=ps, t=t: e.copy(affT[:, 128 * t:128 * t + 128], ps[0:16, 0:128]), r=[ps.key], w=[(affT.key, t)])
        release(m0)

    def stage_M(l):
        m0 = mark()
        aff_tok, affT = state['aff_tok'], state['affT']
        affk = [(affT.key, t) for t in range(32)]
        mask_f = sb([128, 512], F32, 'mask_f')
        mask_b = sb([128, 512], BF16, 'mask_b')
        mscope = mark()
        lo = sb([16, 1], F32, 'lo')
        hi = sb([16, 1], F32, 'hi')
        mid = sb([16, 1], F32, 'mid')
        cnt = sb([16, 1], F32, 'cnt')
        ge = sb([16, 1], F32, 'ge')
        dd = sb([16, 1], F32, 'dd')
        bjunk = sb([16, L], F32, 'bjunk')
        bk = 'bis'
        P.add('dve', lambda e: e.memset(lo[:], 0.0), w=[bk])
        P.add('dve', lambda e: e.memset(hi[:], 1.0), w=[bk])
        for it in range(32):
            P.add('dve', lambda e: e.tensor_tensor(mid[:], lo[:], hi[:], ALU.add), r=[bk], w=[bk])
            P.add('dve', lambda e: e.tensor_scalar(mid[:], mid[:], 0.5, None, ALU.mult), r=[bk], w=[bk])
            P.add('dve', lambda e: e.memset(cnt[:], 0.0), r=[bk], w=[bk])
            P.add('dve', lambda e: e.tensor_scalar(bjunk[:], affT[:], mid[:, 0:1], 0.0, ALU.is_ge, ALU.add, accum_out=cnt[:]), r=[bk] + (affk if it == 0 else []), w=[bk])
            P.add('dve', lambda e: e.tensor_single_scalar(ge[:], cnt[:], 512.0, ALU.is_ge), r=[bk], w=[bk])
            P.add('dve', lambda e: e.tensor_tensor(dd[:], mid[:], lo[:], ALU.subtract), r=[bk], w=[bk])
            P.add('dve', lambda e: e.scalar_tensor_tensor(lo[:], dd[:], ge[:, 0:1], lo[:], ALU.mult, ALU.add), r=[bk], w=[bk])
            P.add('dve', lambda e: e.tensor_tensor(dd[:], hi[:], mid[:], ALU.subtract), r=[bk], w=[bk])
            P.add('dve', lambda e: e.scalar_tensor_tensor(hi[:], dd[:], ge[:, 0:1], mid[:], ALU.mult, ALU.add), r=[bk], w=[bk])
        if dbg_affT is not None:
            dma('sp', dbg_affT, affT[:], [bk], ['dbg_affT'])
            dma('sp', dbg_lo[:, 0:1], lo[:], [bk], ['dbg_lo0'], allow_slow_non_contiguous=True)
            dma('sp', dbg_lo[:, 1:2], hi[:], [bk], ['dbg_lo1'], allow_slow_non_contiguous=True)
            dma('sp', dbg_lo[:, 2:3], cnt[:], [bk], ['dbg_lo2'], allow_slow_non_contiguous=True)
            dma('sp', dbg_lo[:, 3:4], ge[:], [bk], ['dbg_lo3'], allow_slow_non_contiguous=True)
            dma('sp', dbg_afftok, aff_tok[:].rearrange("p t e -> p (t e)"), [(aff_tok.key, t) for t in range(32)], ['dbg_afftok'])
        if dbg.get('mstop', 99) <= 1:
            release(m0)
            return
        maskT = sb([16, L], F32, 'maskT')
        P.add('dve', lambda e: e.tensor_scalar(maskT[:], affT[:], lo[:, 0:1], None, ALU.is_ge), r=[bk], w=[maskT.key])
        if dbg.get('mstop', 99) <= 1.25:
            release(m0)
            return
        ps = ps_next()
        for t in range(32):
            P.add('pe', lambda e, ps=ps, t=t: e.matmul(ps[:, 16 * t:16 * t + 16], maskT[:, 128 * t:128 * t + 128], ident_f[0:16, 0:16], start=True, stop=True), r=[maskT.key, ident_f.key], w=[ps.key])
        if dbg.get('mstop', 99) <= 1.5:
            P.add('dve', lambda e, ps=ps: e.tensor_copy(bjunk[:, 0:512], ps[0:16, :]), r=[ps.key], w=[bjunk.key])
            release(m0)
            return
        P.add('dve', lambda e, ps=ps: e.tensor_copy(mask_f[:], ps[:]), r=[ps.key], w=[mask_f.key])
        P.add('act', lambda e: e.copy(mask_b[:], mask_f[:]), r=[mask_f.key], w=[mask_b.key])
        if dbg_mask is not None:
            dma('sp', dbg_mask, mask_f[:], [mask_f.key], ['dbg_mask'])
            dma('sp', dbg_maskT, maskT[:], [maskT.key], ['dbg_maskT'])
        if dbg.get('mstop', 99) <= 2:
            release(m0)
            return
        release(mscope)
        ut = sb([128, 128], BF16, 'ut')
        ones = sb([128, 128], BF16, 'ones')
        dma('sp', ut[:], C['ut_bf'], [], [ut.key])
        dma('sp', ones[:], C['ones_bf'], [], [ones.key])
        ps_in = ps_next()
        ps_tot = ps_next()
        P.add('pe', lambda e: e.matmul(ps_in[:], ut[:], mask_b[:], start=True, stop=True), r=[ut.key, mask_b.key], w=[ps_in.key])
        P.add('pe', lambda e: e.matmul(ps_tot[:], ones[:], mask_b[:], start=True, stop=True), r=[ones.key, mask_b.key], w=[ps_tot.key])
        tot = sb([128, 32, 16], F32, 'tot')
        offs = sb([128, 32, 16], F32, 'offs')
        posm = sb([128, 32, 16], F32, 'posm')
        P.add('dve', lambda e: e.tensor_copy(tot[:].rearrange("p t e -> p (t e)"), ps_tot[:]), r=[ps_tot.key], w=[tot.key])
        P.add('dve', lambda e: e.memset(offs[:, 0, :], 0.0), w=[offs.key])
        for t in range(31):
            P.add('dve', lambda e, t=t: e.tensor_tensor(offs[:, t + 1, :], offs[:, t, :], tot[:, t, :], ALU.add), r=[offs.key, tot.key], w=[offs.key])
        pf = posm[:].rearrange("p t e -> p (t e)")
        P.add('dve', lambda e: e.tensor_tensor(pf, ps_in[:], offs[:].rearrange("p t e -> p (t e)"), ALU.add), r=[ps_in.key, offs.key], w=[posm.key])
        P.add('dve', lambda e: e.tensor_scalar(pf, pf, 1.0, None, ALU.add), r=[posm.key], w=[posm.key])
        P.add('dve', lambda e: e.tensor_tensor(pf, pf, mask_f[:], ALU.mult), r=[posm.key, mask_f.key], w=[posm.key])
        P.add('dve', lambda e: e.tensor_scalar(pf, pf, -1.0, None, ALU.add), r=[posm.key], w=[posm.key])
        if dbg.get('mstop', 99) <= 3:
            release(m0)
            return
        RH = sb([128, 32, 16, 4], BF16, 'RH')
        tokc = sb([128, 32, 2], BF16, 'tokc')
        dma('sp', tokc[:], C['tokc'], [], [tokc.key])
        ahi = sb([128, 512], BF16, 'ahi')
        ahf = sb([128, 512], F32, 'ahf')
        afk = [(aff_tok.key, t) for t in range(32)]
        aflat = aff_tok[:].rearrange("p t e -> p (t e)")
        P.add('dve', lambda e: e.tensor_copy(ahi[:], aflat), r=afk, w=[ahi.key])
        P.add('dve', lambda e: e.tensor_copy(ahf[:], ahi[:]), r=[ahi.key], w=[ahf.key])
        P.add('dve', lambda e: e.tensor_tensor(ahf[:], aflat, ahf[:], ALU.subtract), r=afk + [ahf.key], w=[ahf.key])
        for ee in range(16):
            P.add('dve', lambda e, ee=ee: e.tensor_copy(RH[:, :, ee, 0:2], tokc[:]), r=[tokc.key], w=[(RH.key, 'a', ee)])
        P.add('dve', lambda e: e.tensor_copy(RH[:, :, :, 2], ahi[:].rearrange("p (t e) -> p t e", t=32)), r=[ahi.key], w=[(RH.key, 'b')])
        P.add('dve', lambda e: e.tensor_copy(RH[:, :, :, 3], ahf[:].rearrange("p (t e) -> p t e", t=32)), r=[ahf.key], w=[(RH.key, 'c')])
        rhk = [(RH.key, 'a', ee) for ee in range(16)] + [(RH.key, 'b'), (RH.key, 'c')]
        if dbg.get('mstop', 99) <= 4:
            release(m0)
            return
        iota = sb([128, 512], F32, 'iota')
        dma('sp', iota[:], C['iota512'], [], [iota.key])
        if dbg_aff is not None:
            dma('sp', dbg_aff, posm[:].rearrange("p t e -> p (t e)"), [posm.key], ['dbg_aff'])
        sels = [sb([128, 512], BF16, 'sel') for _ in range(4)]
        idxg = sb([128, 16], F32, 'idxg')
        r4 = sb([4, 512], F32, 'r4')
        idxf = sb([128, 4], F32, 'idxf')
        idxis = [sb([128, 4], I32, 'idxi') for _ in range(2)]
        gates = [sb([128, 4], F32, 'gate') for _ in range(2)]
        xss = [sb([128, 4, D], BF16, 'xs') for _ in range(2)]
        xsT = sb([128, 8, 512], BF16, 'xsT')
        gT = sb([128, 16, 512], BF16, 'gT')
        sgs = [sb([128, 512], F32, 'sg') for _ in range(2)]
        yos = [sb([128, D], F32, 'yo') for _ in range(2)]
        NSLOT = 8
        wslots = [sb([128, 4096], BF16, 'wslot') for _ in range(NSLOT)]
        wrr = [0]

        def load_piece(src_ap, shape_str, **kw):
            slot = wslots[wrr[0] % NSLOT]
            wrr[0] += 1
            dst = slot[:].rearrange(shape_str, **kw)
            dma('pool', dst, src_ap, [], [slot.key])
            return slot, dst
        h2k = [('h2', t) for t in range(32)]
        selc = 0
        for ex in range(dbg.get('nexp', 16)):
            b = ex % 2
            idxi, gate, xs = idxis[b], gates[b], xss[b]
            ps_r = ps_next()
            for t in range(32):
                sel = sels[selc % 4]
                selc += 1
                P.add('dve', lambda e, sel=sel, t=t, ex=ex: e.tensor_scalar(sel[:], iota[:], posm[:, t, ex:ex + 1], None, ALU.is_equal),
                      r=[iota.key, posm.key], w=[sel.key])
                P.add('pe', lambda e, sel=sel, t=t, ex=ex, ps_r=ps_r: e.matmul(ps_r[0:4, :], RH[:, t, ex, :], sel[:], start=(t == 0), stop=(t == 31)),
                      r=[sel.key] + (rhk if t == 0 else []), w=[ps_r.key])
            P.add('dve', lambda e, ps_r=ps_r: e.tensor_copy(r4[:], ps_r[0:4, :]), r=[ps_r.key], w=[r4.key])
            ps_idx = ps_next()
            for j in range(4):
                P.add('pe', lambda e, j=j, ps_idx=ps_idx: e.matmul(ps_idx[:, 4 * j:4 * j + 4], r4[:, 128 * j:128 * j + 128], ident_f[0:4, 0:4], start=True, stop=True),
                      r=[r4.key, ident_f.key], w=[ps_idx.key])
            P.add('dve', lambda e, ps_idx=ps_idx: e.tensor_copy(idxg[:], ps_idx[:, 0:16]), r=[ps_idx.key], w=[idxg.key])
            if dbg_rh is not None and ex == 0:
                dma('sp', dbg_rh, RH[:].rearrange("p t e c -> p (t e c)"), rhk, ['dbg_rh'])
                dma('sp', dbg_idxg, idxg[:], [idxg.key], ['dbg_idxg'])
            ig = idxg[:].rearrange("p (j c) -> p j c", c=4)
            P.add('dve', lambda e, ig=ig: e.scalar_tensor_tensor(idxf[:], ig[:, :, 0], 128.0, ig[:, :, 1], ALU.mult, ALU.add), r=[idxg.key], w=[idxf.key])
            P.add('dve', lambda e: e.tensor_scalar(idxf[:], idxf[:], 0.0, float(L - 1), ALU.max, ALU.min), r=[idxf.key], w=[idxf.key])
            P.add('dve', lambda e, idxi=idxi: e.tensor_copy(idxi[:], idxf[:]), r=[idxf.key], w=[idxi.key])
            P.add('dve', lambda e, ig=ig, gate=gate: e.tensor_tensor(gate[:], ig[:, :, 2], ig[:, :, 3], ALU.add), r=[idxg.key], w=[gate.key])
            if dbg_idx is not None:
                dma('sp', dbg_idx[:, 4 * ex:4 * ex + 4], idxf[:], [idxf.key], [('dbg_idx', ex)])
            for j in range(4):
                P.add('pool', lambda e, xs=xs, idxi=idxi, j=j: e.indirect_dma_start(out=xs[:, j, :], out_offset=None, in_=h2_d[:, :],
                                                                                     in_offset=bass.IndirectOffsetOnAxis(ap=idxi[:, j:j + 1], axis=0)),
                      r=[idxi.key] + h2k, w=[(xs.key, j)], dma=True)
            for j in range(4):
                for k4 in range(2):
                    ps = ps_next()
                    psb = ps[:].bitcast(BF16)
                    for kk in range(4):
                        k = 4 * k4 + kk
                        P.add('pe', lambda e, psb=psb, xs=xs, j=j, k=k, kk=kk: e.transpose(psb[:, 128 * kk:128 * kk + 128], xs[:, j, 128 * k:128 * k + 128], ident_bf[:]),
                              r=[(xs.key, j), ident_bf.key], w=[ps.key])
                    evac(xsT[:, 4 * k4:4 * k4 + 4, 128 * j:128 * j + 128], psb[:, 0:512].rearrange("p (k t) -> p k t", k=4), [ps.key], [(xsT.key, j, k4)])
            xk = [(xsT.key, j, k4) for j in range(4) for k4 in range(2)]
            for s4 in range(4):
                wg, wgv = load_piece(W['w_gate'][l, ex][:, 512 * s4:512 * s4 + 512].rearrange("(k p) f -> p k f", p=128), "p (k f) -> p k f", k=8)
                wu, wuv = load_piece(W['w_up'][l, ex][:, 512 * s4:512 * s4 + 512].rearrange("(k p) f -> p k f", p=128), "p (k f) -> p k f", k=8)
                for f4 in range(4):
                    fc = 4 * s4 + f4
                    psA, psB = ps_next(), ps_next()
                    sg = sgs[fc % 2]
                    for k in range(8):
                        P.add('pe', lambda e, psA=psA, wgv=wgv, k=k, f4=f4: e.matmul(psA[:], wgv[:, k, 128 * f4:128 * f4 + 128], xsT[:, k, :], start=(k == 0), stop=(k == 7)),
                              r=[wg.key] + (xk if k == 0 else []), w=[psA.key])
                    for k in range(8):
                        P.add('pe', lambda e, psB=psB, wuv=wuv, k=k, f4=f4: e.matmul(psB[:], wuv[:, k, 128 * f4:128 * f4 + 128], xsT[:, k, :], start=(k == 0), stop=(k == 7)),
                              r=[wu.key] + (xk if k == 0 else []), w=[psB.key])
                    P.add('act', lambda e, psA=psA, sg=sg: e.activation(sg[:], psA[:], AF.Silu), r=[psA.key], w=[sg.key])
                    P.add('dve', lambda e, psB=psB, sg=sg, fc=fc: e.tensor_tensor(gT[:, fc, :], psB[:], sg[:], ALU.mult), r=[psB.key, sg.key], w=[(gT.key, fc)])
            wds = []
            for s4 in range(4):
                wd, wdv = load_piece(W['w_down'][l, ex][512 * s4:512 * s4 + 512, :].rearrange("(k p) d -> p k d", p=128), "p (k d) -> p k d", k=4)
                wds.append((wd, wdv))
            gk = [(gT.key, fc) for fc in range(16)]
            for j in range(4):
                yo = yos[j % 2]
                for nh in range(2):
                    ps = ps_next()
                    for fc in range(16):
                        wd, wdv = wds[fc // 4]
                        P.add('pe', lambda e, ps=ps, fc=fc, j=j, nh=nh, wdv=wdv: e.matmul(ps[:], gT[:, fc, 128 * j:128 * j + 128], wdv[:, fc % 4, 512 * nh:512 * nh + 512], start=(fc == 0), stop=(fc == 15)),
                              r=[wd.key] + (gk if fc == 0 else []), w=[ps.key])
                    P.add('act', lambda e, ps=ps, yo=yo, nh=nh, gate=gate, j=j: e.activation(yo[:, 512 * nh:512 * nh + 512], ps[:], AF.Copy, scale=gate[:, j:j + 1]),
                          r=[ps.key, gate.key], w=[(yo.key, nh)])
                P.add('pool', lambda e, yo=yo, idxi=idxi, j=j: e.indirect_dma_start(out=xr[:, :], out_offset=bass.IndirectOffsetOnAxis(ap=idxi[:, j:j + 1], axis=0),
                                                                                     in_=yo[:, :], in_offset=None, compute_op=ALU.add),
                      r=[idxi.key, (yo.key, 0), (yo.key, 1)] + [('xr', t) for t in range(32)], w=['xr_scatter'], dma=True)
        release(m0)

    def stage_F():
        m0 = mark()
        gfb = sb([128, D], F32, 'gfb')
        dma('sp', gfb[:], bcast_rows(W['final_g'][0:1, :], 128, D), [], [gfb.key])
        xts = [sb([128, D], F32, 'xt') for _ in range(2)]
        ots = [sb([128, D], F32, 'ot') for _ in range(2)]
        junk = sb([128, D], F32, 'junk')
        sss = [sb([128, 1], F32, 'ss') for _ in range(2)]
        okeys = []
        for t in range(32):
            xt, ot, ss = xts[t % 2], ots[t % 2], sss[t % 2]
            dma('sp', xt[:], xr[128 * t:128 * t + 128, :], [('xr', t), 'xr_scatter'], [xt.key])
            P.add('act', lambda e, xt=xt, ss=ss: e.activation(junk[:], xt[:], AF.Square, accum_out=ss[:]), r=[xt.key], w=[junk.key, ss.key])
            rstd_from_ss(ss, D)
            P.add('dve', lambda e, xt=xt, ss=ss, ot=ot: e.scalar_tensor_tensor(ot[:], xt[:], ss[:, 0:1], gfb[:], ALU.mult, ALU.mult), r=[xt.key, ss.key, gfb.key], w=[ot.key])
            dma('sp', out[128 * t:128 * t + 128, :], ot[:], [ot.key], [('out', t)])
            okeys.append(('out', t))
        P.add('sp', lambda e: e.nop(), r=okeys + [k for k in dbg_keys])
        release(m0)

    dbg_keys = []
    stages = dbg.get('stages', 'AHNPEM')
    for l in range(n_layers):
        if 'A' in stages:
            stage_A(l)
        if 'H' in stages:
            stage_H(l)
        if 'N' in stages:
            stage_N(l)
        if 'P' in stages:
            stage_P(l)
        if 'E' in stages or 'M' in stages:
            mm = mark()
            state['aff_tok'] = sb([128, 32, 16], F32, 'aff_tok')
            state['affT'] = sb([16, L], F32, 'affT')
            if 'E' in stages:
                stage_E(l)
            if 'M' in stages:
                stage_M(l)
            release(mm)
        if l > 0 or True:
            for t in range(32):
                pass
    if dbg.get('final', True):
        stage_F()
    else:
        allk = set()
        for op in P.ops:
            if op[0] != 'BAR' and op[4]:
                allk.update(op[3])
        P.add('sp', lambda e: e.nop(), r=list(allk))
    nops = P.emit()
    return nc, nops


_CACHE = {}


def kernel(**inputs):
    consts = build_consts()
    nabv = build_na_bias(np.asarray(inputs['na_rpb'], np.float32))
    if 'nc' not in _CACHE:
        _CACHE['nc'] = build_program()[0]
    nc = _CACHE['nc']
    shared = {}
    for k, v in inputs.items():
        if k in ('x', 'na_rpb'):
            continue
        a = np.ascontiguousarray(np.asarray(v, np.float32))
        if k == 'final_g':
            a = a.reshape(1, D)
        shared[k] = a
    shared['nab'] = nabv
    shared.update(consts)
    x = np.asarray(inputs['x'], np.float32)
    in_maps = []
    for c in range(8):
        m = dict(shared)
        m['x'] = np.ascontiguousarray(x[c])
        in_maps.append(m)
    res = run_bass_kernel_spmd(nc, in_maps, core_ids=list(range(8)))
    return np.stack([res.results[c]['out'] for c in range(8)], axis=0).astype(np.float32)
```
